# Optimizing a Trainium2 kernel written in Bass

```python
import math
import jax
import jax.numpy as jnp
from jax import lax
import numpy as np

D_MODEL = 1024
BATCH = 8
SEQ = 2048
DEPTH = 4
DEC_BATCH = 128
DEC_SEQ = 4
PAST_LEN = 16384
PAGE_SIZE = 128

N_HEADS = 4
HEAD_DIM = D_MODEL // N_HEADS
D_INNER = N_HEADS * HEAD_DIM
CHUNK = 128
POOL_WINDOWS = (2, 4, 8, 16)
N_POOL_GROUPS = len(POOL_WINDOWS)
POOL_GROUP_DIM = D_MODEL // N_POOL_GROUPS
POOL_BUF = max(POOL_WINDOWS) - 1
N_MEM = 256
N_XHEADS = 4
XHEAD_DIM = D_MODEL // N_XHEADS
D_FF = ((8 * D_MODEL // 3 + 127) // 128) * 128
N_MLSTM_LAYERS = (DEPTH + 1) // 2
N_POOL_LAYERS = DEPTH // 2
EPS = 1e-6

kernel_name = 'hybrid_mlstm_pool_macaron_memxattn_step'

F32 = jnp.float32


def rmsnorm(x, g):
    xf = x.astype(F32)
    y = xf * lax.rsqrt(jnp.mean(xf * xf, axis=-1, keepdims=True) + EPS) * g.astype(F32)
    return y.astype(x.dtype)


def swiglu(h, w_up, w_down):
    gate, up = jnp.split(h @ w_up, 2, axis=-1)
    return (jax.nn.silu(gate) * up) @ w_down


def mlstm_scan(q, k, v, ig, lf, C, n, m):
    B, H, T, _ = q.shape
    L = CHUNK if T % CHUNK == 0 else T
    nc = T // L
    mask = jnp.tril(jnp.ones((L, L), dtype=bool))

    def to_chunks(a):
        return jnp.moveaxis(a.reshape((B, H, nc, L) + a.shape[3:]), 2, 0)

    def step(carry, inp):
        C, n, m = carry
        qc, kc, vc, ic, fc = inp
        b = jnp.cumsum(fc, axis=-1)
        a = b + m[..., None]
        dlog = b[..., :, None] - b[..., None, :] + ic[..., None, :]
        dlog = jnp.where(mask, dlog, -jnp.inf)
        mt = jnp.maximum(a, jnp.max(dlog, axis=-1))
        dw = jnp.exp(dlog - mt[..., None])
        s = jnp.einsum('bhtd,bhsd->bhts', qc, kc) * dw
        inter = jnp.exp(a - mt)
        num = inter[..., None] * jnp.einsum('bhtd,bhde->bhte', qc, C) + jnp.einsum('bhts,bhse->bhte', s, vc)
        den = inter * jnp.einsum('bhtd,bhd->bht', qc, n) + jnp.sum(s, axis=-1)
        h = num / jnp.maximum(jnp.abs(den), jnp.exp(-mt))[..., None]
        m_new = mt[..., -1]
        decay = jnp.exp(b[..., -1] + m - m_new)
        wk = jnp.exp(b[..., -1:] - b + ic - m_new[..., None])
        C_new = decay[..., None, None] * C + jnp.einsum('bhs,bhsd,bhse->bhde', wk, kc, vc)
        n_new = decay[..., None] * n + jnp.einsum('bhs,bhsd->bhd', wk, kc)
        return (C_new, n_new, m_new), h

    (C, n, m), hs = lax.scan(step, (C, n, m), tuple(to_chunks(a) for a in (q, k, v, ig, lf)))
    hs = jnp.moveaxis(hs, 0, 2).reshape(B, H, T, HEAD_DIM)
    return hs, C, n, m


def mlstm_mixer(h, w_in, b_i, b_f, g_head, w_out, C, n, m):
    B, T, _ = h.shape
    p = (h @ w_in).astype(F32)

    def heads(a):
        return a.reshape(B, T, N_HEADS, HEAD_DIM).transpose(0, 2, 1, 3)

    q = heads(p[..., :D_INNER])
    k = heads(p[..., D_INNER:2 * D_INNER]) * (HEAD_DIM ** -0.5)
    v = heads(p[..., 2 * D_INNER:3 * D_INNER])
    o = p[..., 3 * D_INNER:4 * D_INNER]
    ig = (p[..., 4 * D_INNER:4 * D_INNER + N_HEADS] + b_i.astype(F32)).transpose(0, 2, 1)
    lf = jax.nn.log_sigmoid(p[..., 4 * D_INNER + N_HEADS:] + b_f.astype(F32)).transpose(0, 2, 1)
    hs, C, n, m = mlstm_scan(q, k, v, ig, lf, C.astype(F32), n.astype(F32), m.astype(F32))
    hn = hs * lax.rsqrt(jnp.mean(hs * hs, axis=-1, keepdims=True) + EPS) * g_head.astype(F32)[:, None, :]
    hn = hn.transpose(0, 2, 1, 3).reshape(B, T, D_INNER)
    y = jax.nn.sigmoid(o) * hn
    return (y @ w_out.astype(F32)).astype(h.dtype), C, n, m


def pool_mixer(h, w_in, w_grp, scale, w_out, buf, start_pos):
    T = h.shape[1]
    u = h @ w_in
    ext = jnp.concatenate([buf.astype(u.dtype), u], axis=1)
    ef = ext.astype(F32)
    cs = jnp.concatenate([jnp.zeros_like(ef[:, :1]), jnp.cumsum(ef, axis=1)], axis=1)
    hi = cs[:, POOL_BUF + 1:]
    cnt_pos = start_pos + 1 + jnp.arange(T)
    outs = []
    for g, w in enumerate(POOL_WINDOWS):
        sl = slice(g * POOL_GROUP_DIM, (g + 1) * POOL_GROUP_DIM)
        lo = cs[:, POOL_BUF + 1 - w:POOL_BUF + 1 - w + T, sl]
        cnt = jnp.minimum(cnt_pos, w).astype(F32)[None, :, None]
        d = (hi[..., sl] - lo) / cnt - ef[:, POOL_BUF:, sl]
        outs.append(d @ w_grp[g].astype(F32))
    y = jnp.concatenate(outs, axis=-1) * scale.astype(F32)
    return (y @ w_out.astype(F32)).astype(h.dtype), ext[:, -POOL_BUF:]


def mem_kv(mem, w_kv):
    B, M, _ = mem.shape
    kv = (mem @ w_kv).reshape(B, M, 2, N_XHEADS, XHEAD_DIM)
    return kv[:, :, 0], kv[:, :, 1]


def cross_attn(h, w_q, k, v, w_o):
    B, T, _ = h.shape
    q = (h @ w_q).reshape(B, T, N_XHEADS, XHEAD_DIM)
    s = jnp.einsum('bthd,bmhd->bhtm', q.astype(F32), k.astype(F32)) * (XHEAD_DIM ** -0.5)
    p = jax.nn.softmax(s, axis=-1)
    o = jnp.einsum('bhtm,bmhd->bthd', p, v.astype(F32)).reshape(B, T, N_XHEADS * XHEAD_DIM)
    return (o @ w_o.astype(F32)).astype(h.dtype)


def setup_inputs(seed: int = 0) -> dict:
    key = jax.random.key(seed)
    ks = iter(jax.random.split(key, 32))

    def nrm(shape, scale=1.0):
        return jax.random.normal(next(ks), shape, F32) * scale

    D = D_MODEL
    return {
        'x_prompt': nrm((BATCH, SEQ, D)),
        'x_sample': nrm((DEC_BATCH, DEC_SEQ, D)),
        'mem_prompt': nrm((BATCH, N_MEM, D)),
        'cache_mem_k': nrm((DEPTH, DEC_BATCH, N_MEM, N_XHEADS, XHEAD_DIM)),
        'cache_mem_v': nrm((DEPTH, DEC_BATCH, N_MEM, N_XHEADS, XHEAD_DIM)),
        'state_mlstm_C': nrm((N_MLSTM_LAYERS, DEC_BATCH, N_HEADS, HEAD_DIM, HEAD_DIM), 0.1),
        'state_mlstm_n': nrm((N_MLSTM_LAYERS, DEC_BATCH, N_HEADS, HEAD_DIM), 0.1),
        'state_mlstm_m': nrm((N_MLSTM_LAYERS, DEC_BATCH, N_HEADS), 0.5),
        'state_pool_buf': nrm((N_POOL_LAYERS, DEC_BATCH, POOL_BUF, D)),
        'norm_g': 1.0 + nrm((DEPTH, 4, D), 0.05),
        'final_g': 1.0 + nrm((D,), 0.05),
        'ffn_w_up': nrm((DEPTH, 2, D, 2 * D_FF), D ** -0.5),
        'ffn_w_down': nrm((DEPTH, 2, D_FF, D), D_FF ** -0.5),
        'mlstm_w_in': nrm((N_MLSTM_LAYERS, D, 4 * D_INNER + 2 * N_HEADS), D ** -0.5),
        'mlstm_b_i': nrm((N_MLSTM_LAYERS, N_HEADS), 0.1),
        'mlstm_b_f': 3.0 + nrm((N_MLSTM_LAYERS, N_HEADS), 0.5),
        'mlstm_g_head': 1.0 + nrm((N_MLSTM_LAYERS, N_HEADS, HEAD_DIM), 0.05),
        'mlstm_w_out': nrm((N_MLSTM_LAYERS, D_INNER, D), D_INNER ** -0.5),
        'pool_w_in': nrm((N_POOL_LAYERS, D, D), D ** -0.5),
        'pool_w_grp': nrm((N_POOL_LAYERS, N_POOL_GROUPS, POOL_GROUP_DIM, POOL_GROUP_DIM), POOL_GROUP_DIM ** -0.5),
        'pool_scale': 1.0 + nrm((N_POOL_LAYERS, D), 0.1),
        'pool_w_out': nrm((N_POOL_LAYERS, D, D), D ** -0.5),
        'xattn_w_q': nrm((DEPTH, D, D), D ** -0.5),
        'xattn_w_kv': nrm((DEPTH, D, 2 * D), D ** -0.5),
        'xattn_w_o': nrm((DEPTH, D, D), D ** -0.5),
    }


def reference(x_prompt, x_sample, mem_prompt, cache_mem_k, cache_mem_v, state_mlstm_C, state_mlstm_n,
              state_mlstm_m, state_pool_buf, norm_g, final_g, ffn_w_up, ffn_w_down, mlstm_w_in, mlstm_b_i,
              mlstm_b_f, mlstm_g_head, mlstm_w_out, pool_w_in, pool_w_grp, pool_scale, pool_w_out,
              xattn_w_q, xattn_w_kv, xattn_w_o):
    xp, xs = x_prompt, x_sample
    Bp = xp.shape[0]
    mk_p, mv_p = [], []
    Cp_l, np_l, mp_l, Cs_l, ns_l, ms_l = [], [], [], [], [], []
    bp_l, bs_l = [], []
    for l in range(DEPTH):
        g = norm_g[l]
        j = l // 2
        xp = xp + 0.5 * swiglu(rmsnorm(xp, g[0]), ffn_w_up[l, 0], ffn_w_down[l, 0])
        xs = xs + 0.5 * swiglu(rmsnorm(xs, g[0]), ffn_w_up[l, 0], ffn_w_down[l, 0])
        if l % 2 == 0:
            yp, C1, n1, m1 = mlstm_mixer(rmsnorm(xp, g[1]), mlstm_w_in[j], mlstm_b_i[j], mlstm_b_f[j],
                                         mlstm_g_head[j], mlstm_w_out[j],
                                         jnp.zeros((Bp, N_HEADS, HEAD_DIM, HEAD_DIM), F32),
                                         jnp.zeros((Bp, N_HEADS, HEAD_DIM), F32),
                                         jnp.zeros((Bp, N_HEADS), F32))
            ys, C2, n2, m2 = mlstm_mixer(rmsnorm(xs, g[1]), mlstm_w_in[j], mlstm_b_i[j], mlstm_b_f[j],
                                         mlstm_g_head[j], mlstm_w_out[j],
                                         state_mlstm_C[j], state_mlstm_n[j], state_mlstm_m[j])
            Cp_l.append(C1.astype(x_prompt.dtype)); np_l.append(n1.astype(x_prompt.dtype)); mp_l.append(m1.astype(x_prompt.dtype))
            Cs_l.append(C2.astype(state_mlstm_C.dtype)); ns_l.append(n2.astype(state_mlstm_n.dtype)); ms_l.append(m2.astype(state_mlstm_m.dtype))
        else:
            yp, b1 = pool_mixer(rmsnorm(xp, g[1]), pool_w_in[j], pool_w_grp[j], pool_scale[j], pool_w_out[j],
                                jnp.zeros((Bp, POOL_BUF, D_MODEL), xp.dtype), 0)
            ys, b2 = pool_mixer(rmsnorm(xs, g[1]), pool_w_in[j], pool_w_grp[j], pool_scale[j], pool_w_out[j],
                                state_pool_buf[j], PAST_LEN)
            bp_l.append(b1); bs_l.append(b2.astype(state_pool_buf.dtype))
        xp = xp + yp
        xs = xs + ys
        kp, vp = mem_kv(mem_prompt, xattn_w_kv[l])
        xp = xp + cross_attn(rmsnorm(xp, g[2]), xattn_w_q[l], kp, vp, xattn_w_o[l])
        xs = xs + cross_attn(rmsnorm(xs, g[2]), xattn_w_q[l], cache_mem_k[l], cache_mem_v[l], xattn_w_o[l])
        mk_p.append(kp); mv_p.append(vp)
        xp = xp + 0.5 * swiglu(rmsnorm(xp, g[3]), ffn_w_up[l, 1], ffn_w_down[l, 1])
        xs = xs + 0.5 * swiglu(rmsnorm(xs, g[3]), ffn_w_up[l, 1], ffn_w_down[l, 1])
    y_prompt = rmsnorm(xp, final_g)
    y_sample = rmsnorm(xs, final_g)
    return (y_prompt, y_sample, jnp.stack(mk_p), jnp.stack(mv_p),
            jnp.stack(Cp_l), jnp.stack(np_l), jnp.stack(mp_l),
            jnp.stack(Cs_l), jnp.stack(ns_l), jnp.stack(ms_l),
            jnp.stack(bp_l), jnp.stack(bs_l))
```

```python
import numpy as np
import concourse.bass as bass
import concourse.mybir as mybir
from concourse.bass_utils import run_bass_kernel_spmd

F32 = mybir.dt.float32
BF16 = mybir.dt.bfloat16
AF = mybir.ActivationFunctionType
ALU = mybir.AluOpType
AX = mybir.AxisListType

D = 1024
NPR = 2048
NSM = 64
NT = NPR + NSM
DEPTH = 4
DFF = 2816
NMEM = 256
EPS = 1e-6
TT = [(0, 512), (512, 512), (1024, 512), (1536, 512), (2048, 64)]
NSLOT = 9
SLOT_EL = 2048
ARENA_EL = 26112

FLAGS = {"ffn": True, "mix": True, "xattn": True, "depth": DEPTH}


import sys as _sys


def _ln():
    f = _sys._getframe(2)
    out = []
    while f is not None and len(out) < 4:
        if f.f_code.co_name not in ("op", "dma", "_wait", "mm", "tr", "wload"):
            out.append(f.f_lineno)
        f = f.f_back
    return out


class Dep:
    __slots__ = ("w", "r", "dead")

    def __init__(self):
        self.w = None
        self.r = []
        self.dead = False


class Eng:
    def __init__(self, nc, h, name):
        self.h = h
        self.name = name
        self.sem = nc.alloc_semaphore("prog_" + name)
        self.cnt = 0
        self.waited = {}
        self.log = []


class K:
    def __init__(self, nc):
        self.nc = nc
        self.pe = Eng(nc, nc.tensor, "pe")
        self.act = Eng(nc, nc.scalar, "act")
        self.dve = Eng(nc, nc.vector, "dve")
        self.pool = Eng(nc, nc.gpsimd, "pool")
        self.sp = Eng(nc, nc.sync, "sp")
        self.sems = {}
        for e in (self.pe, self.act, self.dve, self.pool, self.sp):
            self.sems[e.sem.num] = e.sem
        self.dsem = {}
        for qn in ("pool", "sp", "act"):
            lst = []
            for i in range(12):
                s = nc.alloc_semaphore("d_%s_%d" % (qn, i))
                self.sems[s.num] = s
                lst.append([s, 0])
            self.dsem[qn] = [lst, 0]
        self.ps = nc.alloc_psum_tensor("psall", [128, 8, 512], F32)
        self.psd = [Dep() for _ in range(8)]
        self.psi = 0
        self.ring = nc.alloc_sbuf_tensor("ring", [128, NSLOT, SLOT_EL], BF16)
        self.ringd = [Dep() for _ in range(NSLOT)]
        self.ringi = 0

    def _wait(self, E, toks):
        need = {}
        for t in toks:
            if t is None:
                continue
            s, v = t
            if need.get(s, 0) < v:
                need[s] = v
        for s, v in need.items():
            if s == E.sem.num and v > E.cnt:
                continue
            if E.waited.get(s, 0) < v:
                E.h.wait_ge(self.sems[s], v)
                E.waited[s] = v
                E.log.append(("w", s, v, _ln()))

    def _deps(self, R, W):
        toks = []
        for d in R:
            assert not d.dead, "use of a recycled ring slot / psum bank"
            toks.append(d.w)
        for d in W:
            assert not d.dead, "use of a recycled ring slot"
            toks.append(d.w)
            toks.extend(d.r)
        return toks

    def op(self, E, fn, R=(), W=(), inc=True):
        self._wait(E, self._deps(R, W))
        ins = fn()
        if inc:
            E.cnt += 1
            ins.then_inc(E.sem, 1)
            E.log.append(("i", E.sem.num, 1, _ln()))
            tok = (E.sem.num, E.cnt)
        else:
            tok = (E.sem.num, E.cnt + 1)
        for d in R:
            d.r.append(tok)
        for d in W:
            d.w = tok
            d.r = []
        return ins

    def dma(self, E, out, in_, R=(), W=(), **kw):
        pool = self.dsem[E.name]
        lst, i = pool
        ent = lst[i % len(lst)]
        pool[1] = i + 1
        s, c = ent
        toks = self._deps(R, W)
        toks.append((s.num, c))
        self._wait(E, toks)
        ent[1] = c + 16
        E.h.dma_start(out=out, in_=in_, **kw).then_inc(s, 16)
        E.log.append(("i", s.num, 16, _ln()))
        tok = (s.num, c + 16)
        for d in R:
            d.r.append(tok)
        for d in W:
            d.w = tok
            d.r = []

    def finish(self):
        toks = []
        for qn, (lst, _) in self.dsem.items():
            for s, c in lst:
                if c:
                    toks.append((s.num, c))
        for e in (self.pe, self.act, self.dve, self.pool):
            if e.cnt:
                toks.append((e.sem.num, e.cnt))
        self._wait(self.sp, toks)
        self.check_deadlock()

    def check_deadlock(self):
        engs = [self.pe, self.act, self.dve, self.pool, self.sp]
        pos = [0] * len(engs)
        val = {}
        prog = True
        while prog:
            prog = False
            for i, e in enumerate(engs):
                while pos[i] < len(e.log):
                    kind, s_, v, _l = e.log[pos[i]]
                    if kind == "w":
                        if val.get(s_, 0) < v:
                            break
                    else:
                        val[s_] = val.get(s_, 0) + v
                    pos[i] += 1
                    prog = True
        bad = [(e.name, pos[i], len(e.log), e.log[pos[i]], self.sems[e.log[pos[i]][1]].name) for i, e in enumerate(engs) if pos[i] < len(e.log)]
        assert not bad, "DEADLOCK: %s" % (bad,)

    NROT = 6

    def psum_pin(self, j):
        i = self.NROT + j
        return self.ps[:, i, :], [self.psd[i]]

    def _renew(self, i):
        old = self.psd[i]
        new = Dep()
        new.r = list(old.r) + ([old.w] if old.w is not None else [])
        old.dead = True
        self.psd[i] = new

    def psum(self, n=1):
        if n == 2 and self.psi % 2:
            self.psi += 1
        i = self.psi % self.NROT
        self.psi += n
        for q in range(n):
            self._renew(i + q)
        if n == 1:
            return self.ps[:, i, :], [self.psd[i]]
        return self.ps[:, i:i + 2, :], [self.psd[i], self.psd[i + 1]]

    def slot(self):
        i = self.ringi % NSLOT
        self.ringi += 1
        old = self.ringd[i]
        new = Dep()
        new.r = list(old.r) + ([old.w] if old.w is not None else [])
        old.dead = True
        self.ringd[i] = new
        return self.ring[:, i, :], new

    def mm(self, out, lhsT, rhs, start, stop, R, W, inc):
        return self.op(self.pe, lambda: self.nc.tensor.matmul(out, lhsT=lhsT, rhs=rhs, start=start, stop=stop),
                       R, W, inc)

    def tr(self, out, in_, ident, R, W, inc):
        return self.op(self.pe, lambda: self.nc.tensor.transpose(out, in_, ident), R, W, inc)

    def wload(self, src_ap, shape):
        ap, d = self.slot()
        n = 1
        for s in shape[1:]:
            n *= s
        assert n <= SLOT_EL, shape
        v = ap[0:shape[0], 0:n]
        if len(shape) == 3:
            v = v.rearrange("p (a b) -> p a b", a=shape[1])
        self.dma(self.pool, v, src_ap, R=(), W=(d,))
        return v, d


def rows_kc(w_ap, c0, c1):
    return w_ap.rearrange("(kc p) n -> p kc n", p=128)[:, :, c0:c1]


def build():
    nc = bass.Bass("TRN2", target_bir_lowering=False)
    k = K(nc)
    PE, ACT, DVE, POOL, SP = k.pe, k.act, k.dve, k.pool, k.sp
    V = nc.vector
    A = nc.scalar
    G = nc.gpsimd
    nd = FLAGS["depth"]

    def din(name, shape):
        return nc.dram_tensor(name, list(shape), F32, kind="ExternalInput").ap()

    def dout(name, shape):
        return nc.dram_tensor(name, list(shape), F32, kind="ExternalOutput").ap()

    xp_d = din("xp", [NPR, D]); xs_d = din("xs", [NSM, D]); mem_d = din("mem", [NMEM, D])
    ck_d = din("ck", [DEPTH, 16, NMEM, D]); cv_d = din("cv", [DEPTH, 16, NMEM, D])
    sC_d = din("sC", [2, 16, 4, 256, 256]); sn_d = din("sn", [2, 64, 256]); sm_d = din("sm", [2, 16, 4])
    spb_d = din("spb", [2, 240, D])
    vecs_d = din("vecs", [256, 128]); gb_d = din("gb", [4, 4]); cst_d = din("cst", [128, 512])
    rm_d = din("rm", [4, NT])
    w_up_d = din("ffn_w_up", [DEPTH, 2, D, 2 * DFF]); w_dn_d = din("ffn_w_down", [DEPTH, 2, DFF, D])
    mw_in_d = din("mlstm_w_in", [2, D, 4104]); mw_out_d = din("mlstm_w_out", [2, D, D])
    pw_in_d = din("pool_w_in", [2, D, D]); pw_grp_d = din("pool_w_grp", [2, 4, 256, 256]); pw_out_d = din("pool_w_out", [2, D, D])
    xw_q_d = din("xattn_w_q", [DEPTH, D, D]); xw_kv_d = din("xattn_w_kv", [DEPTH, D, 2 * D]); xw_o_d = din("xattn_w_o", [DEPTH, D, D])

    yp_d = dout("yp", [NPR, D]); ys_d = dout("ys", [NSM, D])
    mk_d = dout("mk", [DEPTH, NMEM, D]); mv_d = dout("mv", [DEPTH, NMEM, D])
    Cp_d = dout("Cp", [2, 4, 256, 256]); np_d = dout("np_", [2, 4, 256]); mp_d = dout("mp", [2, 4])
    Cs_d = dout("Cs", [2, 16, 4, 256, 256]); ns_d = dout("ns", [2, 64, 256]); ms_d = dout("ms", [2, 16, 4])
    pbp_d = dout("pbp", [2, 15, D]); pbs_d = dout("pbs", [2, 16, 15, D])

    xres = nc.alloc_sbuf_tensor("xres", [128, 8, NT], F32)
    xres_d = [[Dep() for _ in TT] for _ in range(8)]
    xn = nc.alloc_sbuf_tensor("xn", [128, 8, NT], BF16)
    xn_d = [Dep() for _ in TT]
    arena = nc.alloc_sbuf_tensor("arena", [128, ARENA_EL], BF16)
    cst = nc.alloc_sbuf_tensor("cst_sb", [128, 512], F32)
    cst_dep = Dep()
    identb = nc.alloc_sbuf_tensor("identb", [128, 128], BF16)
    Ub = nc.alloc_sbuf_tensor("Ub", [128, 128], BF16)
    maskbd = nc.alloc_sbuf_tensor("maskbd", [64, 64], BF16)
    onesb = nc.alloc_sbuf_tensor("onesb", [128, 128], BF16)
    rmb = nc.alloc_sbuf_tensor("rmb", [64, 16], BF16)
    ones4 = nc.alloc_sbuf_tensor("ones4", [4, 128], F32)
    VEC = nc.alloc_sbuf_tensor("VEC", [128, 256], F32)
    gb = nc.alloc_sbuf_tensor("gb_sb", [4, 6], F32)
    const_dep = Dep()
    rstd = nc.alloc_sbuf_tensor("rstd", [128, 1, 512], F32)
    rstd_d = [Dep(), Dep()]
    sqbuf = nc.alloc_sbuf_tensor("sqbuf", [128, 4, 512], BF16)
    sqbuf_d = Dep()
    ident = cst[:, 0:128]
    U32 = cst[:, 128:256]

    k.dma(SP, cst[:], cst_d, W=(cst_dep,))
    k.dma(SP, gb[:, 0:4], gb_d, W=(const_dep,))
    k.op(DVE, lambda: V.tensor_copy(out=identb[:], in_=cst[:, 0:128]), R=(cst_dep,), W=(const_dep,))
    k.op(DVE, lambda: V.tensor_copy(out=Ub[:], in_=cst[:, 128:256]), R=(cst_dep,), W=(const_dep,))
    k.op(DVE, lambda: V.tensor_copy(out=maskbd[:], in_=cst[0:64, 256:320]), R=(cst_dep,), W=(const_dep,))
    k.op(DVE, lambda: V.memset(onesb[:], 1.0), W=(const_dep,))
    k.op(DVE, lambda: V.tensor_copy(out=rmb[:], in_=cst[0:64, 320:336]), R=(cst_dep,), W=(const_dep,))
    k.op(DVE, lambda: V.memset(ones4[:], 1.0), W=(const_dep,))
    k.op(DVE, lambda: V.tensor_scalar_mul(out=gb[:, 4:6], in0=gb[:, 2:4], scalar1=-1.0), R=(const_dep,), W=(const_dep,))
    vtmp, vtd = k.slot()
    vt32 = vtmp.bitcast(F32)
    k.dma(SP, vt32[:, 0:256].rearrange("p (a b) -> p a b", a=2), vecs_d.rearrange("(a p) n -> p a n", p=128), W=(vtd,))
    pv, pvd = k.psum()
    for a in range(2):
        k.tr(pv[:, a * 128:(a + 1) * 128], vt32[:, a * 128:(a + 1) * 128], ident, R=(vtd, cst_dep), W=pvd, inc=(a == 1))
    k.op(DVE, lambda: V.tensor_copy(out=VEC[:], in_=pv[:, 0:256]), R=pvd, W=(const_dep,))

    if FLAGS.get("stop") == 1:
        k.finish(); return nc

    def ng_col(l, i, c):
        j = (l * 4 + i) * 8 + c
        return VEC[:, j:j + 1]

    def load_transpose(src_rows_ap, nrows, col0):
        st, sd = k.slot()
        st2, sd2 = k.slot()
        s32a = st.bitcast(F32)
        s32b = st2.bitcast(F32)
        k.dma(SP, s32a[0:nrows, 0:512], src_rows_ap[:, 0:512], W=(sd,))
        k.dma(SP, s32b[0:nrows, 0:512], src_rows_ap[:, 512:1024], W=(sd2,))
        for hlf, (s32, dd) in enumerate(((s32a, sd), (s32b, sd2))):
            pt, ptd = k.psum()
            for c4 in range(4):
                k.tr(pt[:, c4 * 128:c4 * 128 + nrows], s32[0:nrows, c4 * 128:(c4 + 1) * 128], ident[0:nrows, 0:nrows],
                     R=(dd, cst_dep), W=ptd, inc=(c4 == 3))
            tti = min(col0 // 512, 4)
            wd = [xres_d[hlf * 4 + c4][tti] for c4 in range(4)]
            src = pt.rearrange("p (c t) -> p c t", c=4)[:, :, 0:nrows]
            eng = ACT if hlf == 0 else DVE
            if eng is ACT:
                k.op(ACT, lambda: A.copy(out=xres[:, 0:4, col0:col0 + nrows], in_=src), R=ptd, W=wd)
            else:
                k.op(DVE, lambda: V.tensor_copy(out=xres[:, 4:8, col0:col0 + nrows], in_=src), R=ptd, W=wd)

    for t in range(16):
        load_transpose(xp_d[t * 128:(t + 1) * 128, :], 128, t * 128)
    load_transpose(xs_d, 64, NPR)

    if FLAGS.get("stop") == 2:
        k.finish(); return nc
    def rmsnorm_part1(tti):
        t0, n = TT[tti]
        pr, prd = k.psum()
        for hlf in range(2):
            sqv, sqd = sqbuf, sqbuf_d
            k.op(ACT, lambda: A.activation(out=sqv[:, :, 0:n], in_=xres[:, hlf * 4:hlf * 4 + 4, t0:t0 + n], func=AF.Square),
                 R=[xres_d[hlf * 4 + c][tti] for c in range(4)], W=(sqd,))
            for c in range(4):
                k.mm(pr[:, 0:n], onesb[:], sqv[:, c, 0:n], start=(hlf == 0 and c == 0), stop=(hlf == 1 and c == 3),
                     R=(sqd, const_dep), W=prd, inc=(c == 3))
        ri = 0
        rs = rstd[:, ri, 0:n]
        k.op(ACT, lambda: A.activation(out=rs, in_=pr[:, 0:n], func=AF.Sqrt, scale=1.0 / D, bias=cst[:, 448:449]),
             R=prd + [cst_dep], W=(rstd_d[ri],))

    def rmsnorm_part2(tti, gcol_fn, out_fn):
        t0, n = TT[tti]
        ri = 0
        rs = rstd[:, ri, 0:n]
        k.op(DVE, lambda: V.reciprocal(out=rs, in_=rs), R=(), W=(rstd_d[ri],))
        for c in range(8):
            oap, odeps = out_fn(c)
            k.op(DVE, lambda: V.scalar_tensor_tensor(out=oap, in0=xres[:, c, t0:t0 + n], scalar=gcol_fn(c), in1=rs,
                                                     op0=ALU.mult, op1=ALU.mult),
                 R=(xres_d[c][tti], rstd_d[ri], const_dep), W=odeps)

    def rmsnorm_tile(tti, gcol_fn, out_fn, out_deps_fn):
        rmsnorm_part1(tti)
        rmsnorm_part2(tti, gcol_fn, out_fn)

    norm_state = {"done": set(), "next": None, "pending": None}

    def flush_pending():
        p = norm_state["pending"]
        if p is not None:
            l_, i_, t_ = p
            t0, n = TT[t_]
            rmsnorm_part2(t_, lambda c: ng_col(l_, i_, c), lambda c: (xn[:, c, t0:t0 + n], (xn_d[t_],)))
            norm_state["done"].add(p)
            norm_state["pending"] = None

    def norm_tile_xn(l, i, tti):
        t0, n = TT[tti]
        rmsnorm_tile(tti, lambda c: ng_col(l, i, c), lambda c: (xn[:, c, t0:t0 + n], (xn_d[tti],)), None)

    def norm_to_xn(l, i):
        flush_pending()
        for tti in range(len(TT)):
            if (l, i, tti) not in norm_state["done"]:
                norm_tile_xn(l, i, tti)

    def early_norm(tti):
        nx = norm_state["next"]
        if nx is not None and FLAGS.get("early_norm", True):
            flush_pending()
            rmsnorm_part1(tti)
            norm_state["pending"] = (nx[0], nx[1], tti)

    arena_live = []

    def arena_deps(shape):
        toks = []
        for d in arena_live:
            if d.w is not None:
                toks.append(d.w)
            toks.extend(d.r)
        dd = {}
        for t in toks:
            if dd.get(t[0], 0) < t[1]:
                dd[t[0]] = t[1]
        toks = list(dd.items())
        del arena_live[:]

        def mk(sh):
            if not sh:
                d = Dep()
                d.r = list(toks)
                arena_live.append(d)
                return d
            return [mk(sh[1:]) for _ in range(sh[0])]
        return mk(list(shape))

    act = arena[:, 0:12 * NT].rearrange("p (c t) -> p c t", c=12)

    def ffn(l, i):
        act_d = arena_deps([12, len(TT)])
        norm_to_xn(l, 0 if i == 0 else 3)
        wu = w_up_d[l, i]
        wd_ = w_dn_d[l, i]
        for half, units in enumerate((range(0, 6), range(6, 11))):
            nchunk = len(units) * 2
            for ui, u in enumerate(units):
                Gp, Gd = k.wload(rows_kc(wu, u * 256, (u + 1) * 256), [128, 8, 256])
                Up, Ud = k.wload(rows_kc(wu, DFF + u * 256, DFF + (u + 1) * 256), [128, 8, 256])
                for tti, (t0, n) in enumerate(TT):
                    for jj in range(2):
                        cl = ui * 2 + jj
                        pg, pgd = k.psum()
                        for kc in range(8):
                            k.mm(pg[:, 0:n], Gp[:, kc, jj * 128:(jj + 1) * 128], xn[:, kc, t0:t0 + n], kc == 0, kc == 7,
                                 R=(Gd, xn_d[tti]), W=pgd, inc=(kc == 7))
                        pu, pud = k.psum()
                        for kc in range(8):
                            k.mm(pu[:, 0:n], Up[:, kc, jj * 128:(jj + 1) * 128], xn[:, kc, t0:t0 + n], kc == 0, kc == 7,
                                 R=(Ud, xn_d[tti]), W=pud, inc=(kc == 7))
                        k.op(ACT, lambda: A.activation(out=act[:, cl, t0:t0 + n], in_=pg[:, 0:n], func=AF.Silu), R=pgd, W=(act_d[cl][tti],))
                        k.op(DVE, lambda: V.tensor_tensor(out=act[:, cl, t0:t0 + n], in0=pu[:, 0:n], in1=act[:, cl, t0:t0 + n], op=ALU.mult),
                             R=pud, W=(act_d[cl][tti],))
            Dps = []
            for ui, u in enumerate(units):
                Dp, Dd = k.wload(wd_[u * 256:(u + 1) * 256, :].rearrange("(a p) n -> p a n", p=128), [128, 2, 1024])
                Dps.append((Dp, Dd))
            for tti, (t0, n) in enumerate(TT):
                for m in range(8):
                    po, pod = k.psum()
                    for cl in range(nchunk):
                        Dp, Dd = Dps[cl // 2]
                        k.mm(po[:, 0:n], Dp[:, cl % 2, m * 128:(m + 1) * 128], act[:, cl, t0:t0 + n], cl == 0, cl == nchunk - 1,
                             R=(Dd, act_d[cl][tti]), W=pod, inc=(cl == nchunk - 1))
                    k.op(DVE, lambda: V.scalar_tensor_tensor(out=xres[:, m, t0:t0 + n], in0=po[:, 0:n], scalar=0.5,
                                                             in1=xres[:, m, t0:t0 + n], op0=ALU.mult, op1=ALU.add),
                         R=pod, W=(xres_d[m][tti],))
                if half == 1:
                    early_norm(tti)

    memT = nc.alloc_sbuf_tensor("memT", [128, 8, NMEM], BF16)
    memT_d = Dep()
    for mc in range(2):
        for hlf in range(2):
            st, sd = k.slot()
            s32 = st.bitcast(F32)
            k.dma(SP, s32[:, 0:512], mem_d[mc * 128:(mc + 1) * 128, hlf * 512:(hlf + 1) * 512], W=(sd,))
            pt, ptd = k.psum()
            for c4 in range(4):
                k.tr(pt[:, c4 * 128:(c4 + 1) * 128], s32[:, c4 * 128:(c4 + 1) * 128], ident, R=(sd, cst_dep), W=ptd, inc=(c4 == 3))
            k.op(ACT, lambda: A.copy(out=memT[:, hlf * 4:hlf * 4 + 4, mc * 128:(mc + 1) * 128],
                                     in_=pt.rearrange("p (c t) -> p c t", c=4)), R=ptd, W=(memT_d,))

    def xattn(l):
        QO = 8 * NT
        qT = arena[:, 0:QO].rearrange("p (c t) -> p c t", c=8)
        KT = arena[:, QO:QO + 2048].rearrange("p (c m) -> p c m", c=8)
        Vp = arena[:, QO + 2048:QO + 4096].rearrange("p (c m) -> p c m", c=2)
        rsS = arena[:, QO + 4096:QO + 4096 + 512].bitcast(F32)
        dl = arena_deps([12])
        qT_d, KT_d, Vp_d, rsS_d = dl[0:8], dl[8], dl[9], dl[10]
        qT_td = [[Dep() for _ in TT] for _ in range(8)]
        for c in range(8):
            for tti in range(len(TT)):
                qT_td[c][tti].r = list(qT_d[c].r)
                arena_live.append(qT_td[c][tti])
        norm_to_xn(l, 2)
        wkv = xw_kv_d[l]
        if FLAGS.get("xstop") == 0:
            return
        for pi in range(8):
            Wp, Wd = k.wload(rows_kc(wkv, pi * 256, (pi + 1) * 256), [128, 8, 256])
            h = pi % 4
            if pi < 4:
                for dc in range(2):
                    ps, psd = k.psum()
                    for kc in range(8):
                        k.mm(ps[:, 0:256], Wp[:, kc, dc * 128:(dc + 1) * 128], memT[:, kc, :], kc == 0, kc == 7,
                             R=(Wd, memT_d), W=psd, inc=(kc == 7))
                    k.op(ACT, lambda: A.copy(out=KT[:, h * 2 + dc, :], in_=ps[:, 0:256]), R=psd, W=(KT_d,))
            for mc in range(2):
                ps, psd = k.psum()
                for kc in range(8):
                    k.mm(ps[:, 0:256], memT[:, kc, mc * 128:(mc + 1) * 128], Wp[:, kc, :], kc == 0, kc == 7,
                         R=(Wd, memT_d), W=psd, inc=(kc == 7))
                if True:
                    stg, stgd = k.slot()
                    st32 = stg.bitcast(F32)[:, 0:256]
                    k.op(DVE, lambda: V.tensor_copy(out=st32, in_=ps[:, 0:256]), R=psd, W=(stgd,))
                    if pi >= 4:
                        k.op(ACT, lambda: A.copy(out=Vp[:, mc, h * 256:(h + 1) * 256], in_=st32), R=(stgd,), W=(Vp_d,))
                    dst = (mk_d if pi < 4 else mv_d)[l, mc * 128:(mc + 1) * 128, h * 256:(h + 1) * 256]
                    k.dma(SP, dst, st32, R=(stgd,))
        if FLAGS.get("xstop") == 1:
            return
        wq = xw_q_d[l]
        for pi in range(4):
            Wp, Wd = k.wload(rows_kc(wq, pi * 256, (pi + 1) * 256), [128, 8, 256])
            for tti, (t0, n) in enumerate(TT):
                for oc in range(2):
                    c = pi * 2 + oc
                    ps, psd = k.psum()
                    for kc in range(8):
                        k.mm(ps[:, 0:n], Wp[:, kc, oc * 128:(oc + 1) * 128], xn[:, kc, t0:t0 + n], kc == 0, kc == 7,
                             R=(Wd, xn_d[tti]), W=psd, inc=(kc == 7))
                    k.op(ACT, lambda: A.mul(out=qT[:, c, t0:t0 + n], in_=ps[:, 0:n], mul=1.0 / 16.0), R=psd, W=(qT_td[c][tti],))
        if FLAGS.get("xstop") == 2:
            return
        oT = xn
        def prompt_block(h, tti):
            t0, n = TT[tti]
            est, esd = k.slot()
            eT = est[:, 0:1024].rearrange("p (c t) -> p c t", c=2)
            for mc in range(2):
                ps, psd = k.psum()
                for dc in range(2):
                    k.mm(ps[:, :], KT[:, h * 2 + dc, mc * 128:(mc + 1) * 128], qT[:, h * 2 + dc, t0:t0 + n], dc == 0, dc == 1,
                         R=(KT_d, qT_td[h * 2 + dc][tti]), W=psd, inc=(dc == 1))
                k.op(ACT, lambda: A.activation(out=eT[:, mc, :], in_=ps[:, :], func=AF.Exp), R=psd, W=(esd,))
            pss, pssd = k.psum()
            for mc in range(2):
                k.mm(pss[:, :], onesb[:], eT[:, mc, :], mc == 0, mc == 1, R=(esd, const_dep), W=pssd, inc=(mc == 1))
            rst, rsd = k.slot()
            rs32 = rst.bitcast(F32)[:, 0:512]
            k.op(DVE, lambda: V.reciprocal(out=rs32, in_=pss[:, :]), R=pssd, W=(rsd,))
            for dc in range(2):
                ps, psd = k.psum()
                for mc in range(2):
                    k.mm(ps[:, :], Vp[:, mc, h * 256 + dc * 128:h * 256 + (dc + 1) * 128], eT[:, mc, :], mc == 0, mc == 1,
                         R=(Vp_d, esd), W=psd, inc=(mc == 1))
                k.op(DVE, lambda: V.tensor_tensor(out=oT[:, h * 2 + dc, t0:t0 + n], in0=ps[:, :], in1=rs32, op=ALU.mult),
                     R=psd + [rsd], W=(xn_d[tti],))
        psS, psSd = k.psum_pin(0)

        def sample_a(b):
            KTb, KTbd = k.slot()
            KTv = KTb.rearrange("p (c m) -> p c m", c=8)
            Kb, Kbd = k.wload(ck_d[l, b].rearrange("(mc p) d -> p mc d", p=128), [128, 2, 1024])
            for half in range(2):
                pt, ptd = k.psum()
                ptb = pt.bitcast(BF16)
                for q4 in range(4):
                    hd = half * 4 + q4
                    for mc in range(2):
                        k.tr(ptb[:, q4 * 256 + mc * 128:q4 * 256 + (mc + 1) * 128], Kb[:, mc, hd * 128:(hd + 1) * 128], identb[:],
                             R=(Kbd, const_dep), W=ptd, inc=(q4 == 3 and mc == 1))
                if half == 0:
                    k.op(ACT, lambda: A.copy(out=KTb[:, 0:1024], in_=ptb[:, 0:1024]), R=ptd, W=(KTbd,))
                else:
                    k.op(DVE, lambda: V.tensor_copy(out=KTb[:, 1024:2048], in_=ptb[:, 0:1024]), R=ptd, W=(KTbd,))
            for h in range(4):
                for mc in range(2):
                    col = b * 32 + (h * 2 + mc) * 4
                    for dc in range(2):
                        k.mm(psS[:, col:col + 4], KTv[:, h * 2 + dc, mc * 128:(mc + 1) * 128],
                             qT[:, h * 2 + dc, NPR + 4 * b:NPR + 4 * b + 4], dc == 0, dc == 1,
                             R=(KTbd, qT_td[h * 2 + dc][4]), W=psSd, inc=(h == 3 and mc == 1 and dc == 1))
        for idx in range(16):
            prompt_block(idx // 4, idx % 4)
            sample_a(idx)
        eS = arena[:, QO + 5120:QO + 5120 + 512]
        eSd = dl[11]
        k.op(ACT, lambda: A.activation(out=eS, in_=psS[:, :], func=AF.Exp), R=psSd, W=(eSd,))
        eS5 = eS.rearrange("p (b h mc t) -> p b h mc t", b=16, h=4, mc=2)
        pss, pssd = k.psum()
        for b in range(16):
            for mc in range(2):
                k.mm(pss[:, b * 16:(b + 1) * 16], onesb[:], eS5[:, b, :, mc, :], mc == 0, mc == 1, R=(eSd, const_dep), W=pssd,
                     inc=(b == 15 and mc == 1))
        k.op(DVE, lambda: V.reciprocal(out=rsS[:, :], in_=pss[:, 0:256]), R=pssd, W=(rsS_d,))
        if FLAGS.get("xstop") == 4:
            return
        wo = xw_o_d[l]

        def wo_tiles(tiles):
            Wps = [k.wload(rows_kc(wo, pi * 256, (pi + 1) * 256), [128, 8, 256]) for pi in range(4)]
            for tti in tiles:
                t0, n = TT[tti]
                for m in range(8):
                    Wp, Wd = Wps[m // 2]
                    oc = m % 2
                    ps, psd = k.psum()
                    for kc in range(8):
                        k.mm(ps[:, 0:n], Wp[:, kc, oc * 128:(oc + 1) * 128], oT[:, kc, t0:t0 + n], kc == 0, kc == 7,
                             R=(Wd, xn_d[tti]), W=psd, inc=(kc == 7))
                    k.op(DVE, lambda: V.tensor_tensor(out=xres[:, m, t0:t0 + n], in0=ps[:, 0:n], in1=xres[:, m, t0:t0 + n], op=ALU.add),
                         R=psd, W=(xres_d[m][tti],))
                early_norm(tti)

        wo_tiles([0, 1, 2, 3])
        psO, psOd = k.psum_pin(1)
        psO4 = psO.rearrange("p (hd b t) -> p hd b t", hd=8, b=16)
        for b in range(16):
            Vb, Vbd = k.wload(cv_d[l, b].rearrange("(mc p) d -> p mc d", p=128), [128, 2, 1024])
            for hd in range(8):
                h = hd // 2
                for mc in range(2):
                    k.mm(psO4[:, hd, b, :], Vb[:, mc, hd * 128:(hd + 1) * 128], eS5[:, b, h, mc, :], mc == 0, mc == 1,
                         R=(Vbd, eSd), W=psOd, inc=(hd == 7 and mc == 1))
        rs4 = rsS.rearrange("p (b h t) -> p h b t", b=16, h=4)
        for hd in range(8):
            o_ap = oT[:, hd, NPR:NT].rearrange("p (b t) -> p b t", b=16)
            i_ap = psO4[:, hd, :, :]
            k.op(DVE, lambda: V.tensor_tensor(out=o_ap, in0=i_ap, in1=rs4[:, hd // 2, :, :], op=ALU.mult), R=psOd + [rsS_d], W=(xn_d[4],))
        wo_tiles([4])

    tmp16 = nc.alloc_sbuf_tensor("tmp16", [128, 2, 16], F32)
    tmp16_d = Dep()

    def pool_mixer(l):
        j = l // 2
        PW = 15 + NPR
        FW = PW + 16 * 19
        bufs = []
        for x in range(2):
            off = x * 2 * FW * 2
            t32 = arena[:, off:off + 2 * FW * 2].bitcast(F32).rearrange("p (c w) -> p c w", c=2)
            bufs.append((t32[:, :, 0:PW], t32[:, :, PW:FW].rearrange("p c (b i) -> p c b i", b=16)))
        uoff = 2 * 2 * FW * 2
        ub = arena[:, uoff:uoff + 2 * NT].rearrange("p (c t) -> p c t", c=2)
        woff = uoff + 2 * NT
        dl = arena_deps([5])
        AB_d = [dl[0], dl[1]]
        ub_d = dl[2]
        Wgd, Wod = dl[3], dl[4]
        norm_to_xn(l, 1)
        k.dma(SP, pbs_d[j, :, 0:11, :], spb_d[j].rearrange("(b i) d -> b i d", i=15)[:, 4:15, :])
        for g in range(4):
            w = 2 << g
            Win, Wind = k.wload(rows_kc(pw_in_d[j], g * 256, (g + 1) * 256), [128, 8, 256])
            Wg = arena[:, woff:woff + 512].rearrange("p (a n) -> p a n", a=2)
            Wo = arena[:, woff + 512:woff + 2560].rearrange("p (a n) -> p a n", a=2)
            k.dma(POOL, Wg, pw_grp_d[j, g].rearrange("(a p) n -> p a n", p=128), W=(Wgd,))
            k.dma(POOL, Wo, pw_out_d[j][g * 256:(g + 1) * 256, :].rearrange("(a p) n -> p a n", p=128), W=(Wod,))
            (Ap, As), (Bp, Bs) = bufs
            k.op(DVE, lambda: V.memset(Ap[:, :, 0:15], 0.0), W=(AB_d[0],))
            k.op(DVE, lambda: V.memset(Bp[:, :, 0:15], 0.0), W=(AB_d[1],))
            for cc in range(2):
                pt, ptd = k.psum()
                for rt, nr in ((0, 128), (1, 112)):
                    st, sd = k.slot()
                    s32 = st.bitcast(F32)
                    k.dma(SP, s32[0:nr, 0:128], spb_d[j, rt * 128:rt * 128 + nr, g * 256 + cc * 128:g * 256 + (cc + 1) * 128], W=(sd,))
                    k.tr(pt[:, rt * 128:rt * 128 + nr], s32[0:nr, 0:128], ident[0:nr, 0:nr], R=(sd, cst_dep), W=ptd, inc=(rt == 1))
                k.op(ACT, lambda: A.copy(out=As[:, cc, :, 0:15], in_=pt[:, 0:240].rearrange("p (b i) -> p b i", b=16)),
                     R=ptd, W=(AB_d[0],))
            for tti, (t0, n) in enumerate(TT):
                for cc in range(2):
                    ps, psd = k.psum()
                    for kc in range(8):
                        k.mm(ps[:, 0:n], Win[:, kc, cc * 128:(cc + 1) * 128], xn[:, kc, t0:t0 + n], kc == 0, kc == 7,
                             R=(Wind, xn_d[tti]), W=psd, inc=(kc == 7))
                    if tti < 4:
                        k.op(ACT, lambda: A.copy(out=Ap[:, cc, 15 + t0:15 + t0 + n], in_=ps[:, 0:n]), R=psd, W=(AB_d[0],))
                        k.op(DVE, lambda: V.tensor_copy(out=ub[:, cc, t0:t0 + n], in_=Ap[:, cc, 15 + t0:15 + t0 + n]),
                             R=(AB_d[0],), W=(ub_d,))
                    else:
                        k.op(ACT, lambda: A.copy(out=As[:, cc, :, 15:19], in_=ps[:, 0:64].rearrange("p (b t) -> p b t", b=16)),
                             R=psd, W=(AB_d[0],))
                        k.op(DVE, lambda: V.tensor_copy(out=ub[:, cc, NPR:NT].rearrange("p (b t) -> p b t", b=16), in_=As[:, cc, :, 15:19]),
                             R=(AB_d[0],), W=(ub_d,))
            for (c0, mrows, which) in ((NPR - 15, 15, 0), (NPR, 64, 1)):
                ps, psd = k.psum()
                for kc in range(8):
                    k.mm(ps[0:mrows, 0:256], xn[:, kc, c0:c0 + mrows], Win[:, kc, :], kc == 0, kc == 7,
                         R=(Wind, xn_d[3 if which == 0 else 4]), W=psd, inc=(kc == 7))
                stg, stgd = k.slot()
                st32 = stg.bitcast(F32)
                k.op(DVE, lambda: V.tensor_copy(out=st32[0:mrows, 0:256], in_=ps[0:mrows, 0:256]), R=psd, W=(stgd,))
                if which == 0:
                    k.dma(SP, pbp_d[j, :, g * 256:(g + 1) * 256], st32[0:15, 0:256], R=(stgd,))
                else:
                    for b in range(16):
                        k.dma(SP, pbs_d[j, b, 11:15, g * 256:(g + 1) * 256], st32[4 * b:4 * b + 4, 0:256], R=(stgd,))
            cur = 0
            for lev in range(g + 1):
                sh = 1 << lev
                (ip, is_), (op_, os_) = bufs[cur], bufs[1 - cur]
                k.op(DVE, lambda: V.tensor_tensor(out=op_[:, :, sh:PW], in0=ip[:, :, sh:PW], in1=ip[:, :, 0:PW - sh], op=ALU.add),
                     R=(AB_d[cur],), W=(AB_d[1 - cur],))
                k.op(DVE, lambda: V.tensor_tensor(out=os_[:, :, :, sh:19], in0=is_[:, :, :, sh:19], in1=is_[:, :, :, 0:19 - sh], op=ALU.add),
                     R=(AB_d[cur],), W=(AB_d[1 - cur],))
                cur = 1 - cur
            wp, ws = bufs[cur]
            wd_ = AB_d[cur]
            invc = cst[:, 384 + g * 16:384 + (g + 1) * 16]
            k.op(DVE, lambda: V.tensor_tensor(out=tmp16[:], in0=wp[:, :, 15:31], in1=invc.unsqueeze(1).to_broadcast([128, 2, 16]), op=ALU.mult),
                 R=(wd_, cst_dep), W=(tmp16_d,))
            k.op(DVE, lambda: V.tensor_tensor(out=ub[:, :, 0:16], in0=tmp16[:], in1=ub[:, :, 0:16], op=ALU.subtract),
                 R=(tmp16_d,), W=(ub_d,))
            k.op(DVE, lambda: V.scalar_tensor_tensor(out=ub[:, :, 16:NPR], in0=wp[:, :, 31:PW], scalar=1.0 / w, in1=ub[:, :, 16:NPR],
                                                     op0=ALU.mult, op1=ALU.subtract), R=(wd_,), W=(ub_d,))
            for cc in range(2):
                us = ub[:, cc, NPR:NT].rearrange("p (b t) -> p b t", b=16)
                k.op(DVE, lambda: V.scalar_tensor_tensor(out=us, in0=ws[:, cc, :, 15:19], scalar=1.0 / w, in1=us,
                                                         op0=ALU.mult, op1=ALU.subtract), R=(wd_,), W=(ub_d,))
            for tti, (t0, n) in enumerate(TT):
                yt, ytd = k.slot()
                ytv = yt[:, 0:1024].rearrange("p (c t) -> p c t", c=2)
                for oc in range(2):
                    ps, psd = k.psum()
                    for kc2 in range(2):
                        k.mm(ps[:, 0:n], Wg[:, kc2, oc * 128:(oc + 1) * 128], ub[:, kc2, t0:t0 + n], kc2 == 0, kc2 == 1,
                             R=(Wgd, ub_d), W=psd, inc=(kc2 == 1))
                    sc = VEC[:, 136 + j * 8 + g * 2 + oc:137 + j * 8 + g * 2 + oc]
                    k.op(ACT, lambda: A.mul(out=ytv[:, oc, 0:n], in_=ps[:, 0:n], mul=sc), R=psd + [const_dep], W=(ytd,))
                for m in range(8):
                    ps, psd = k.psum()
                    for kc2 in range(2):
                        k.mm(ps[:, 0:n], Wo[:, kc2, m * 128:(m + 1) * 128], ytv[:, kc2, 0:n], kc2 == 0, kc2 == 1,
                             R=(Wod, ytd), W=psd, inc=(kc2 == 1))
                    k.op(DVE, lambda: V.tensor_tensor(out=xres[:, m, t0:t0 + n], in0=ps[:, 0:n], in1=xres[:, m, t0:t0 + n], op=ALU.add),
                         R=psd, W=(xres_d[m][tti],))
                if g == 3:
                    early_norm(tti)

    WT = nc.alloc_sbuf_tensor("WT", [128, 17, 8], F32)
    DECB = nc.alloc_sbuf_tensor("DECB", [128, 128], F32)
    Cst = nc.alloc_sbuf_tensor("Cst", [128, 2, 257], F32)
    numS = nc.alloc_sbuf_tensor("numS", [64, 257], F32)
    NST = nc.alloc_sbuf_tensor("NST", [128, 2, 64], F32)
    NSTn = nc.alloc_sbuf_tensor("NSTn", [128, 2, 64], F32)
    RR = nc.alloc_sbuf_tensor("RR", [128, 17], F32)
    SQ = nc.alloc_sbuf_tensor("SQ", [128, 17], F32)
    sml = nc.alloc_sbuf_tensor("sml", [4, 256], F32)
    DDt = nc.alloc_sbuf_tensor("DDt", [4, 128], F32)
    sptb = nc.alloc_sbuf_tensor("sptb", [128, 2, 128], BF16)
    sptb_d = [Dep(), Dep()]

    def mlstm_mixer(l):
        j = l // 2
        win = mw_in_d[j]
        dl = arena_deps([4])
        GI_d, GL_d, GB_d, RM_d = dl
        GI = arena[0:4, 0:2 * NT].bitcast(F32)
        GL = arena[0:4, 2 * NT:4 * NT].bitcast(F32)
        GB = arena[0:4, 4 * NT:6 * NT].bitcast(F32)
        RM = arena[0:4, 6 * NT:8 * NT].bitcast(F32)
        WT_d, DECB_d, Cst_d, numS_d, NST_d, NSTn_d, RR_d, SQ_d, sml_d = [Dep() for _ in range(9)]
        norm_to_xn(l, 1)
        k.dma(SP, RM, rm_d, W=(RM_d,))
        GM, BL, MM, CC, DEC, MS, MN = (sml[:, 0:32], sml[:, 32:64], sml[:, 64:97], sml[:, 100:132], sml[:, 132:164],
                                       sml[:, 164:180], sml[:, 180:196])
        k.dma(SP, MS, sm_d[j].rearrange("b h -> h b"), W=(sml_d,), allow_slow_non_contiguous=True)
        st, sd = k.slot()
        s32 = st.bitcast(F32)
        k.dma(SP, s32[0:64, 0:256], sn_d[j], W=(sd,))
        pt, ptd = k.psum()
        for dc in range(2):
            k.tr(pt[:, dc * 64:(dc + 1) * 64], s32[0:64, dc * 128:(dc + 1) * 128], ident[0:64, 0:64], R=(sd, cst_dep), W=ptd, inc=(dc == 1))
        k.op(DVE, lambda: V.tensor_copy(out=NST[:], in_=pt[:, 0:128].rearrange("p (c n) -> p c n", c=2)), R=ptd, W=(NST_d,))
        Wgt, Wgtd = k.wload(rows_kc(win, 4096, 4104), [128, 8, 8])
        for tti, (t0, n) in enumerate(TT):
            ps, psd = k.psum()
            for kc in range(8):
                k.mm(ps[0:4, 0:n], Wgt[:, kc, 0:4], xn[:, kc, t0:t0 + n], kc == 0, kc == 7, R=(Wgtd, xn_d[tti]), W=psd, inc=(kc == 7))
            k.op(ACT, lambda: A.activation(out=GI[:, t0:t0 + n], in_=ps[0:4, 0:n], func=AF.Identity, bias=gb[:, j:j + 1]),
                 R=psd + [const_dep], W=(GI_d,))
            ps, psd = k.psum()
            for kc in range(8):
                k.mm(ps[0:4, 0:n], Wgt[:, kc, 4:8], xn[:, kc, t0:t0 + n], kc == 0, kc == 7, R=(Wgtd, xn_d[tti]), W=psd, inc=(kc == 7))
            k.op(ACT, lambda: A.activation(out=GL[:, t0:t0 + n], in_=ps[0:4, 0:n], func=AF.Exp, scale=-1.0, bias=gb[:, 4 + j:5 + j]),
                 R=psd + [const_dep], W=(GL_d,))
        k.op(ACT, lambda: A.activation(out=GL, in_=GL, func=AF.Ln, bias=1.0), W=(GL_d,))
        k.op(DVE, lambda: V.tensor_tensor_scan(out=GB, data0=RM, data1=GL, initial=0.0, op0=ALU.mult, op1=ALU.add),
             R=(RM_d, GL_d), W=(GB_d,))
        k.op(DVE, lambda: V.tensor_tensor(out=GI, in0=GI, in1=GB, op=ALU.add), R=(GB_d,), W=(GI_d,))
        GIp = GI[:, 0:NPR].rearrange("p (c t) -> p c t", t=128)
        GIs = GI[:, NPR:NT].rearrange("p (c t) -> p c t", t=4)
        GBp = GB[:, 0:NPR].rearrange("p (c t) -> p c t", t=128)
        GBs = GB[:, NPR:NT].rearrange("p (c t) -> p c t", t=4)
        k.op(DVE, lambda: V.reduce_max(out=GM[:, 0:16], in_=GIp, axis=AX.X), R=(GI_d,), W=(sml_d,))
        k.op(DVE, lambda: V.reduce_max(out=GM[:, 16:32], in_=GIs, axis=AX.X), R=(GI_d,), W=(sml_d,))
        k.op(DVE, lambda: V.tensor_scalar_mul(out=BL[:, 0:16], in0=GBp[:, :, 127], scalar1=-1.0), R=(GB_d,), W=(sml_d,))
        k.op(DVE, lambda: V.tensor_scalar_mul(out=BL[:, 16:32], in0=GBs[:, :, 3], scalar1=-1.0), R=(GB_d,), W=(sml_d,))
        k.op(DVE, lambda: V.memset(MM[:, 0:1], 0.0), W=(sml_d,))
        k.op(DVE, lambda: V.tensor_tensor_scan(out=MM[:, 1:17], data0=GM[:, 0:16], data1=BL[:, 0:16], initial=0.0,
                                               op0=ALU.max, op1=ALU.add), W=(sml_d,))
        k.op(DVE, lambda: V.tensor_tensor(out=CC[:, 0:16], in0=MM[:, 0:16], in1=GM[:, 0:16], op=ALU.max), W=(sml_d,))
        k.op(DVE, lambda: V.tensor_tensor(out=CC[:, 16:32], in0=MS, in1=GM[:, 16:32], op=ALU.max), W=(sml_d,))
        k.op(DVE, lambda: V.tensor_tensor(out=MN, in0=CC[:, 16:32], in1=BL[:, 16:32], op=ALU.add), W=(sml_d,))
        k.op(DVE, lambda: V.tensor_tensor(out=DEC[:, 0:16], in0=MM[:, 0:16], in1=CC[:, 0:16], op=ALU.subtract), W=(sml_d,))
        k.op(DVE, lambda: V.tensor_tensor(out=DEC[:, 16:32], in0=MS, in1=CC[:, 16:32], op=ALU.subtract), W=(sml_d,))
        k.op(ACT, lambda: A.activation(out=DEC, in_=DEC, func=AF.Exp), W=(sml_d,))
        k.dma(SP, mp_d[j].rearrange("(h o) -> h o", o=1), MM[:, 16:17], R=(sml_d,))
        k.dma(SP, ms_d[j].rearrange("b h -> h b"), MN, R=(sml_d,), allow_slow_non_contiguous=True)
        for (Xp, Xs, Xd) in ((GIp, GIs, GI_d), (GBp, GBs, GB_d)):
            k.op(DVE, lambda: V.tensor_tensor(out=Xp, in0=Xp, in1=CC[:, 0:16].unsqueeze(2).to_broadcast([4, 16, 128]), op=ALU.subtract),
                 R=(sml_d,), W=(Xd,))
            k.op(DVE, lambda: V.tensor_tensor(out=Xs, in0=Xs, in1=CC[:, 16:32].unsqueeze(2).to_broadcast([4, 16, 4]), op=ALU.subtract),
                 R=(sml_d,), W=(Xd,))
        k.op(ACT, lambda: A.activation(out=GI, in_=GI, func=AF.Exp), W=(GI_d,))
        k.op(ACT, lambda: A.activation(out=GB, in_=GB, func=AF.Exp), W=(GB_d,))
        pT, pTd = k.psum()
        for tt in range(17):
            n = 128 if tt < 16 else 64
            k.tr(pT[0:n, tt * 8:tt * 8 + 4], GI[:, tt * 128:tt * 128 + n], ident[0:4, 0:4], R=(GI_d, cst_dep), W=pTd, inc=False)
            k.tr(pT[0:n, tt * 8 + 4:tt * 8 + 8], GB[:, tt * 128:tt * 128 + n], ident[0:4, 0:4], R=(GB_d, cst_dep), W=pTd, inc=(tt == 16))
        k.op(DVE, lambda: V.tensor_copy(out=WT[:, 0:16, :], in_=pT[:, 0:128].rearrange("p (t c) -> p t c", c=8)), R=pTd, W=(WT_d,))
        k.op(DVE, lambda: V.tensor_copy(out=WT[0:64, 16, :], in_=pT[0:64, 128:136]), R=pTd, W=(WT_d,))
        k.op(DVE, lambda: V.tensor_tensor(out=DDt[:].rearrange("p (g h) -> p g h", h=4), in0=DEC.unsqueeze(2).to_broadcast([4, 32, 4]),
                                          in1=ident[0:4, 0:4].unsqueeze(1).to_broadcast([4, 32, 4]), op=ALU.mult),
             R=(sml_d, cst_dep), W=(sml_d,))
        pD, pDd = k.psum()
        k.mm(pD[:, 0:128], ones4[:], DDt[:], True, True, R=(sml_d, const_dep), W=pDd, inc=True)
        k.op(DVE, lambda: V.tensor_copy(out=DECB[:], in_=pD[:, 0:128]), R=pDd, W=(DECB_d,))
        o_ = 0
        qT = arena[:, o_:o_ + 2 * NT].rearrange("p (c t) -> p c t", c=2); o_ += 2 * NT
        kT = arena[:, o_:o_ + 2 * NT].rearrange("p (c t) -> p c t", c=2); o_ += 2 * NT
        soT = arena[:, o_:o_ + 2 * NT].rearrange("p (c t) -> p c t", c=2); o_ += 2 * NT
        VX = arena[:, o_:o_ + 17 * 257].rearrange("p (t e) -> p t e", t=17); o_ += 17 * 257 + 1
        KW = arena[:, o_:o_ + 17 * 256].rearrange("p (t e) -> p t e", t=17); o_ += 17 * 256
        HN = arena[:, o_:o_ + 17 * 256].rearrange("p (t e) -> p t e", t=17); o_ += 17 * 256
        assert o_ <= ARENA_EL
        dl = arena_deps([6, 17])
        qT_d, kT_d, so_d = dl[0][0:5], dl[1][0:5], dl[2][0:5]
        VX_d, KW_d, HN_d = dl[3], dl[4], dl[5]
        for tt in range(17):
            k.op(DVE, lambda: V.memset(VX[:, tt, 256:257], 1.0), W=(VX_d[tt],))
        tile_of = lambda tt: (tt * 128, 128 if tt < 16 else 64, min(tt // 4, 4))
        for h in range(4):
            Wq, Wqd = k.wload(rows_kc(win, h * 256, (h + 1) * 256), [128, 8, 256])
            for tti, (t0, n) in enumerate(TT):
                for dc in range(2):
                    ps, psd = k.psum()
                    for kc in range(8):
                        k.mm(ps[:, 0:n], Wq[:, kc, dc * 128:(dc + 1) * 128], xn[:, kc, t0:t0 + n], kc == 0, kc == 7,
                             R=(Wqd, xn_d[tti]), W=psd, inc=(kc == 7))
                    k.op(ACT, lambda: A.copy(out=qT[:, dc, t0:t0 + n], in_=ps[:, 0:n]), R=psd, W=(qT_d[tti],))
            Wk, Wkd = k.wload(rows_kc(win, 1024 + h * 256, 1024 + (h + 1) * 256), [128, 8, 256])
            for tti, (t0, n) in enumerate(TT):
                for dc in range(2):
                    ps, psd = k.psum()
                    for kc in range(8):
                        k.mm(ps[:, 0:n], Wk[:, kc, dc * 128:(dc + 1) * 128], xn[:, kc, t0:t0 + n], kc == 0, kc == 7,
                             R=(Wkd, xn_d[tti]), W=psd, inc=(kc == 7))
                    k.op(ACT, lambda: A.mul(out=kT[:, dc, t0:t0 + n], in_=ps[:, 0:n], mul=1.0 / 16.0), R=psd, W=(kT_d[tti],))
            for tt in range(17):
                c0, n, tti = tile_of(tt)
                ps, psd = k.psum()
                for kc in range(8):
                    k.mm(ps[0:n, 0:256], xn[:, kc, c0:c0 + n], Wk[:, kc, :], kc == 0, kc == 7, R=(Wkd, xn_d[tti]), W=psd, inc=(kc == 7))
                k.op(DVE, lambda: V.tensor_scalar(out=KW[0:n, tt, :], in0=ps[0:n, 0:256], scalar1=WT[0:n, tt, h:h + 1], scalar2=1.0 / 16.0,
                                                  op0=ALU.mult, op1=ALU.mult), R=psd + [WT_d], W=(KW_d[tt],))
            Wv, Wvd = k.wload(rows_kc(win, 2048 + h * 256, 2048 + (h + 1) * 256), [128, 8, 256])
            for tt in range(17):
                c0, n, tti = tile_of(tt)
                ps, psd = k.psum()
                for kc in range(8):
                    k.mm(ps[0:n, 0:256], xn[:, kc, c0:c0 + n], Wv[:, kc, :], kc == 0, kc == 7, R=(Wvd, xn_d[tti]), W=psd, inc=(kc == 7))
                k.op(ACT, lambda: A.copy(out=VX[0:n, tt, 0:256], in_=ps[0:n, 0:256]), R=psd, W=(VX_d[tt],))
            Wo_, Wod_ = k.wload(rows_kc(win, 3072 + h * 256, 3072 + (h + 1) * 256), [128, 8, 256])
            for tti, (t0, n) in enumerate(TT):
                for dc in range(2):
                    ps, psd = k.psum()
                    for kc in range(8):
                        k.mm(ps[:, 0:n], Wo_[:, kc, dc * 128:(dc + 1) * 128], xn[:, kc, t0:t0 + n], kc == 0, kc == 7,
                             R=(Wod_, xn_d[tti]), W=psd, inc=(kc == 7))
                    k.op(ACT, lambda: A.activation(out=soT[:, dc, t0:t0 + n], in_=ps[:, 0:n], func=AF.Sigmoid), R=psd, W=(so_d[tti],))
            def stage_a(j2):
                c0 = j2 * 128
                tti = j2 // 4
                pS, pSd = k.psum()
                for dc in range(2):
                    k.mm(pS[:, 0:128], kT[:, dc, c0:c0 + 128], qT[:, dc, c0:c0 + 128], dc == 0, dc == 1,
                         R=(kT_d[tti], qT_d[tti]), W=pSd, inc=(dc == 1))
                SpT, sptd = sptb[:, j2 % 2, :], sptb_d[j2 % 2]
                k.op(DVE, lambda: V.scalar_tensor_tensor(out=SpT, in0=pS[:, 0:128], scalar=WT[:, j2, h:h + 1], in1=Ub[:],
                                                         op0=ALU.mult, op1=ALU.mult), R=pSd + [WT_d, const_dep], W=(sptd,))
                pC, pCd = k.psum(2)
                for dc in range(2):
                    k.mm(pC[:, dc, 0:257], KW[:, j2, dc * 128:(dc + 1) * 128], VX[:, j2, :], True, True,
                         R=(KW_d[j2], VX_d[j2]), W=pCd, inc=(dc == 1))
                return SpT, sptd, pC, pCd

            cb_next = {}

            def stage_b(j2, st_a):
                SpT, sptd, pC, pCd = st_a
                c0 = j2 * 128
                tti = j2 // 4
                dcol = DECB[:, j2 * 4 + h:j2 * 4 + h + 1]
                if j2 == 0:
                    k.op(DVE, lambda: V.tensor_copy(out=Cst[:], in_=pC[:, :, 0:257]), R=pCd, W=(Cst_d,))
                else:
                    k.op(DVE, lambda: V.scalar_tensor_tensor(out=Cst[:], in0=Cst[:], scalar=dcol, in1=pC[:, :, 0:257],
                                                             op0=ALU.mult, op1=ALU.add), R=pCd + [DECB_d], W=(Cst_d,))
                if j2 > 0:
                    CB, cbd = cb_next[j2]
                if j2 + 1 < 16:
                    dnx = DECB[:, (j2 + 1) * 4 + h:(j2 + 1) * 4 + h + 1]
                    cbt, cbd_n = k.slot()
                    CBn = cbt[:, 0:514].rearrange("p (c e) -> p c e", c=2)
                    k.op(ACT, lambda: A.mul(out=CBn, in_=Cst[:], mul=dnx), R=(Cst_d, DECB_d), W=(cbd_n,))
                    cb_next[j2 + 1] = (CBn, cbd_n)
                pN, pNd = k.psum()
                k.mm(pN[:, 0:257], SpT, VX[:, j2, :], True, j2 == 0, R=(sptd, VX_d[j2]), W=pNd, inc=(j2 == 0))
                if j2 > 0:
                    for dc in range(2):
                        k.mm(pN[:, 0:257], qT[:, dc, c0:c0 + 128], CB[:, dc, :], False, dc == 1, R=(qT_d[tti], cbd), W=pNd, inc=(dc == 1))
                rr_ = RR[:, j2:j2 + 1]
                k.op(DVE, lambda: V.tensor_scalar_mul(out=rr_, in0=pN[:, 256:257], scalar1=-1.0), R=pNd, W=(RR_d,))
                k.op(DVE, lambda: V.tensor_tensor(out=rr_, in0=pN[:, 256:257], in1=rr_, op=ALU.max), R=pNd, W=(RR_d,))
                k.op(DVE, lambda: V.tensor_tensor(out=rr_, in0=rr_, in1=WT[:, j2, 4 + h:5 + h], op=ALU.max), R=(WT_d,), W=(RR_d,))
                k.op(DVE, lambda: V.reciprocal(out=rr_, in_=rr_), W=(RR_d,))
                k.op(ACT, lambda: A.mul(out=HN[:, j2, :], in_=pN[:, 0:256], mul=rr_), R=pNd + [RR_d], W=(HN_d[j2],))

            sc0 = NPR
            pS, pSd = k.psum()
            for dc in range(2):
                k.mm(pS[0:64, 0:64], kT[:, dc, sc0:NT], qT[:, dc, sc0:NT], dc == 0, dc == 1, R=(kT_d[4], qT_d[4]), W=pSd, inc=(dc == 1))
            spt, sptd = k.slot()
            SpTs = spt[0:64, 0:64]
            k.op(DVE, lambda: V.scalar_tensor_tensor(out=SpTs, in0=pS[0:64, 0:64], scalar=WT[0:64, 16, h:h + 1], in1=maskbd[:],
                                                     op0=ALU.mult, op1=ALU.mult), R=pSd + [WT_d, const_dep], W=(sptd,))
            pN, pNd = k.psum()
            k.mm(pN[0:64, 0:257], SpTs, VX[0:64, 16, :], True, True, R=(sptd, VX_d[16]), W=pNd, inc=True)
            k.op(DVE, lambda: V.tensor_copy(out=numS[:], in_=pN[0:64, 0:257]), R=pNd, W=(numS_d,))
            st_next = stage_a(0)

            pDN, pDNd = k.psum()
            for dc in range(2):
                k.mm(pDN[:, dc * 16:(dc + 1) * 16], KW[0:64, 16, dc * 128:(dc + 1) * 128], rmb[:], True, True,
                     R=(KW_d[16], const_dep), W=pDNd, inc=(dc == 1))
            for dc in range(2):
                nv = NSTn[:, dc, :].rearrange("p (b h) -> p b h", h=4)[:, :, h]
                k.op(DVE, lambda: V.tensor_tensor(out=nv, in0=NST[:, dc, :].rearrange("p (b h) -> p b h", h=4)[:, :, h],
                                                  in1=DECB[:, 64:128].rearrange("p (b h) -> p b h", h=4)[:, :, h], op=ALU.mult),
                     R=(NST_d, DECB_d), W=(NSTn_d,))
                k.op(DVE, lambda: V.tensor_tensor(out=nv, in0=pDN[:, dc * 16:(dc + 1) * 16], in1=nv, op=ALU.add), R=pDNd, W=(NSTn_d,))

            def sample_front(b):
                dcol = DECB[:, (16 + b) * 4 + h:(16 + b) * 4 + h + 1]
                rmk = cst[0:64, 320 + b:321 + b]
                cbt_, cbd_ = k.slot()
                Cb = cbt_.bitcast(F32)[:, 0:514].rearrange("p (c e) -> p c e", c=2)
                k.dma(SP, Cb[:, :, 0:256], sC_d[j, b, h].rearrange("(dc p) e -> p dc e", p=128), W=(cbd_,))
                k.op(ACT, lambda: A.copy(out=Cb[:, :, 256:257], in_=NST[:, :, b * 4 + h:b * 4 + h + 1]), R=(NST_d,), W=(cbd_,))
                cbb, cbbd = k.slot()
                CBb = cbb[:, 0:514].rearrange("p (c e) -> p c e", c=2)
                k.op(ACT, lambda: A.mul(out=CBb, in_=Cb, mul=dcol), R=(cbd_, DECB_d), W=(cbbd,))
                vxt, vxd = k.slot()
                VXb = vxt[0:64, 0:256]
                k.op(ACT, lambda: A.mul(out=VXb, in_=VX[0:64, 16, 0:256], mul=rmk), R=(VX_d[16], cst_dep), W=(vxd,))
                return (Cb, cbd_, CBb, cbbd, VXb, vxd, dcol, rmk)

            def sample_back(b, fr):
                Cb, cbd_, CBb, cbbd, VXb, vxd, dcol, rmk = fr
                pb, pbd = k.psum_pin(0)
                for dc in range(2):
                    k.mm(pb[0:64, 0:257], qT[:, dc, sc0:NT], CBb[:, dc, :], dc == 0, dc == 1, R=(qT_d[4], cbbd), W=pbd, inc=(dc == 1))
                k.op(DVE, lambda: V.scalar_tensor_tensor(out=numS[:], in0=pb[0:64, 0:257], scalar=rmk, in1=numS[:],
                                                         op0=ALU.mult, op1=ALU.add), R=pbd + [cst_dep], W=(numS_d,))
                pC1, pC1d = k.psum_pin(1)
                for dc in range(2):
                    k.mm(pC1[:, dc * 256:(dc + 1) * 256], KW[0:64, 16, dc * 128:(dc + 1) * 128], VXb, True, True,
                         R=(KW_d[16], vxd), W=pC1d, inc=(dc == 1))
                cnt_, cnd_ = k.slot()
                Cn = cnt_.bitcast(F32)[:, 0:512].rearrange("p (c e) -> p c e", c=2)
                k.op(DVE, lambda: V.scalar_tensor_tensor(out=Cn, in0=Cb[:, :, 0:256], scalar=dcol, in1=pC1.rearrange("p (c e) -> p c e", c=2),
                                                         op0=ALU.mult, op1=ALU.add), R=pC1d + [cbd_, DECB_d], W=(cnd_,))
                k.dma(POOL, Cs_d[j, b, h].rearrange("(dc p) e -> p dc e", p=128), Cn, R=(cnd_,))

            fr_prev = None
            for j2 in range(16):
                st_cur = st_next
                if j2 + 1 < 16:
                    st_next = stage_a(j2 + 1)
                if fr_prev is not None:
                    sample_back(j2 - 1, fr_prev)
                fr_prev = sample_front(j2)
                stage_b(j2, st_cur)
            sample_back(15, fr_prev)
            k.dma(POOL, Cp_d[j, h].rearrange("(dc p) e -> p dc e", p=128), Cst[:, :, 0:256], R=(Cst_d,))
            k.dma(POOL, np_d[j, h].rearrange("(dc p o) -> p dc o", p=128, o=1), Cst[:, :, 256:257], R=(Cst_d,), allow_slow_non_contiguous=True)
            rs_ = RR[0:64, 16:17]
            k.op(DVE, lambda: V.tensor_scalar_mul(out=rs_, in0=numS[:, 256:257], scalar1=-1.0), R=(numS_d,), W=(RR_d,))
            k.op(DVE, lambda: V.tensor_tensor(out=rs_, in0=numS[:, 256:257], in1=rs_, op=ALU.max), R=(numS_d,), W=(RR_d,))
            k.op(DVE, lambda: V.tensor_tensor(out=rs_, in0=rs_, in1=WT[0:64, 16, 4 + h:5 + h], op=ALU.max), R=(WT_d,), W=(RR_d,))
            k.op(DVE, lambda: V.reciprocal(out=RR[0:64, 16:17], in_=RR[0:64, 16:17]), W=(RR_d,))
            k.op(ACT, lambda: A.mul(out=HN[0:64, 16, :], in_=numS[:, 0:256], mul=RR[0:64, 16:17]), R=(numS_d, RR_d), W=(HN_d[16],))
            k.op(DVE, lambda: V.memset(SQ[:], 0.0), W=(SQ_d,))
            for tt in range(17):
                n = 128 if tt < 16 else 64
                jt, jtd = k.slot()
                jb = jt[0:n, 0:256]
                k.op(DVE, lambda: V.scalar_tensor_tensor(out=jb, in0=HN[0:n, tt, :], scalar=1.0, in1=HN[0:n, tt, :], op0=ALU.mult, op1=ALU.mult,
                                                         accum_out=SQ[0:n, tt:tt + 1]), R=(HN_d[tt],), W=(jtd, SQ_d))
            for (r0, r1, c0_, c1_) in ((0, 128, 0, 16), (0, 64, 16, 17)):
                sq = SQ[r0:r1, c0_:c1_]
                k.op(DVE, lambda: V.tensor_scalar(out=sq, in0=sq, scalar1=1.0 / 256.0, scalar2=EPS, op0=ALU.mult, op1=ALU.add), W=(SQ_d,))
                k.op(ACT, lambda: A.sqrt(out=sq, in_=sq), W=(SQ_d,))
                k.op(DVE, lambda: V.reciprocal(out=sq, in_=sq), W=(SQ_d,))
            for ec in range(2):
                gh = VEC[:, 152 + j * 8 + h * 2 + ec:153 + j * 8 + h * 2 + ec]
                for grp in range(5):
                    tts = list(range(grp * 4, grp * 4 + 4)) if grp < 4 else [16]
                    pt, ptd = k.psum()
                    ptb = pt.bitcast(BF16)
                    hs = []
                    for idx, tt in enumerate(tts):
                        n = 128 if tt < 16 else 64
                        hsl, hsd = k.slot()
                        hv = hsl[0:n, 0:128]
                        k.op(ACT, lambda: A.mul(out=hv, in_=HN[0:n, tt, ec * 128:(ec + 1) * 128], mul=SQ[0:n, tt:tt + 1]),
                             R=(HN_d[tt], SQ_d), W=(hsd,))
                        k.tr(ptb[:, idx * 128:idx * 128 + n], hv, identb[0:n, 0:n], R=(hsd, const_dep), W=ptd, inc=(idx == len(tts) - 1))
                    t0, n = TT[grp]
                    k.op(DVE, lambda: V.scalar_tensor_tensor(out=soT[:, ec, t0:t0 + n], in0=ptb[:, 0:n], scalar=gh, in1=soT[:, ec, t0:t0 + n],
                                                             op0=ALU.mult, op1=ALU.mult), R=ptd + [const_dep], W=(so_d[grp],))
            Wout, Woutd = k.wload(mw_out_d[j][h * 256:(h + 1) * 256, :].rearrange("(a p) n -> p a n", p=128), [128, 2, 1024])
            for tti, (t0, n) in enumerate(TT):
                for m in range(8):
                    ps, psd = k.psum()
                    for dc in range(2):
                        k.mm(ps[:, 0:n], Wout[:, dc, m * 128:(m + 1) * 128], soT[:, dc, t0:t0 + n], dc == 0, dc == 1,
                             R=(Woutd, so_d[tti]), W=psd, inc=(dc == 1))
                    k.op(DVE, lambda: V.tensor_tensor(out=xres[:, m, t0:t0 + n], in0=ps[:, 0:n], in1=xres[:, m, t0:t0 + n], op=ALU.add),
                         R=psd, W=(xres_d[m][tti],))
                if h == 3:
                    early_norm(tti)
        pt, ptd = k.psum()
        for dc in range(2):
            k.tr(pt[0:64, dc * 128:(dc + 1) * 128], NSTn[:, dc, :], ident, R=(NSTn_d, cst_dep), W=ptd, inc=(dc == 1))
        st, sd = k.slot()
        s32 = st.bitcast(F32)
        k.op(DVE, lambda: V.tensor_copy(out=s32[0:64, 0:256], in_=pt[0:64, 0:256]), R=ptd, W=(sd,))
        k.dma(SP, ns_d[j], s32[0:64, 0:256], R=(sd,))

    subs = []
    for l in range(nd):
        if FLAGS["ffn"]:
            subs.append((lambda l=l: ffn(l, 0), (l, 0)))
        if FLAGS["mix"] and l % 2 == 0 and FLAGS.get("mlstm", True):
            subs.append((lambda l=l: mlstm_mixer(l), (l, 1)))
        if FLAGS["mix"] and l % 2 == 1 and FLAGS.get("pool", True):
            subs.append((lambda l=l: pool_mixer(l), (l, 1)))
        if FLAGS["xattn"]:
            subs.append((lambda l=l: xattn(l), (l, 2)))
        if FLAGS["ffn"]:
            subs.append((lambda l=l: ffn(l, 1), (l, 3)))
    for si, (fn, nid) in enumerate(subs):
        norm_state["next"] = subs[si + 1][1] if si + 1 < len(subs) else None
        fn()

    fin = arena[:, 0:2 * 8 * 512].bitcast(F32).rearrange("p (c t) -> p c t", c=8)
    flush_pending()
    fin_d = arena_deps([1])[0]
    for tti, (t0, n) in enumerate(TT):
        rmsnorm_tile(tti, lambda c: VEC[:, 128 + c:129 + c], lambda c: (fin[:, c, 0:n], (fin_d,)), None)
        for s in range((n + 127) // 128):
            nr = min(128, n - s * 128)
            ost, osd = k.slot()
            ost2, osd2 = k.slot()
            for hlf, (o_, od_) in enumerate(((ost, osd), (ost2, osd2))):
                o32 = o_.bitcast(F32)
                pt, ptd = k.psum()
                for c4 in range(4):
                    k.tr(pt[0:nr, c4 * 128:(c4 + 1) * 128], fin[:, hlf * 4 + c4, s * 128:s * 128 + nr], ident,
                         R=(fin_d, cst_dep), W=ptd, inc=(c4 == 3))
                if hlf == 0:
                    k.op(ACT, lambda: A.copy(out=o32[0:nr, 0:512], in_=pt[0:nr, :]), R=ptd, W=(od_,))
                else:
                    k.op(DVE, lambda: V.tensor_copy(out=o32[0:nr, 0:512], in_=pt[0:nr, :]), R=ptd, W=(od_,))
                r0 = t0 + s * 128
                dst = yp_d[r0:r0 + nr, hlf * 512:(hlf + 1) * 512] if r0 < NPR else ys_d[0:nr, hlf * 512:(hlf + 1) * 512]
                k.dma(SP, dst, o32[0:nr, 0:512], R=(od_,))
    k.finish()
    return nc


_OUT_NAMES = ["yp", "ys", "mk", "mv", "Cp", "np_", "mp", "Cs", "ns", "ms", "pbp", "pbs"]


def _consts():
    cst = np.zeros((128, 512), np.float32)
    cst[:, 0:128] = np.eye(128, dtype=np.float32)
    s = np.arange(128)
    cst[:, 128:256] = (s[:, None] <= s[None, :]).astype(np.float32)
    s64 = np.arange(64)
    cst[0:64, 256:320] = ((s64[:, None] // 4 == s64[None, :] // 4) & (s64[:, None] <= s64[None, :])).astype(np.float32)
    cst[0:64, 320:336] = (s64[:, None] // 4 == np.arange(16)[None, :]).astype(np.float32)
    for g, w in enumerate((2, 4, 8, 16)):
        cst[:, 384 + g * 16:384 + (g + 1) * 16] = 1.0 / np.minimum(np.arange(16) + 1, w)
    cst[:, 448] = EPS
    rm = np.ones((4, NT), np.float32)
    rm[:, 0:NPR:128] = 0.0
    rm[:, NPR::4] = 0.0
    return cst, rm


def kernel(**inp):
    f = lambda a: np.ascontiguousarray(np.asarray(a, dtype=np.float32))
    cst, rm = _consts()
    vecs = np.zeros((256, 128), np.float32)
    vecs[0:128] = f(inp["norm_g"]).reshape(128, 128)
    vecs[128:136] = f(inp["final_g"]).reshape(8, 128)
    vecs[136:152] = f(inp["pool_scale"]).reshape(16, 128)
    vecs[152:168] = f(inp["mlstm_g_head"]).reshape(16, 128)
    gb = np.ascontiguousarray(np.concatenate([f(inp["mlstm_b_i"]).T, f(inp["mlstm_b_f"]).T], axis=1))
    shared = {n: f(inp[n]) for n in ("ffn_w_up", "ffn_w_down", "mlstm_w_in", "mlstm_w_out", "pool_w_in", "pool_w_grp",
                                      "pool_w_out", "xattn_w_q", "xattn_w_kv", "xattn_w_o")}
    shared.update(vecs=vecs, gb=gb, cst=cst, rm=rm)
    in_maps = []
    for c in range(8):
        b0 = 16 * c
        m = dict(shared)
        m["xp"] = f(inp["x_prompt"][c])
        m["xs"] = f(inp["x_sample"][b0:b0 + 16]).reshape(NSM, D)
        m["mem"] = f(inp["mem_prompt"][c])
        m["ck"] = f(inp["cache_mem_k"][:, b0:b0 + 16]).reshape(DEPTH, 16, NMEM, D)
        m["cv"] = f(inp["cache_mem_v"][:, b0:b0 + 16]).reshape(DEPTH, 16, NMEM, D)
        m["sC"] = f(inp["state_mlstm_C"][:, b0:b0 + 16])
        m["sn"] = f(inp["state_mlstm_n"][:, b0:b0 + 16]).reshape(2, 64, 256)
        m["sm"] = f(inp["state_mlstm_m"][:, b0:b0 + 16])
        m["spb"] = f(inp["state_pool_buf"][:, b0:b0 + 16]).reshape(2, 240, D)
        in_maps.append(m)
    nc = build()
    res = run_bass_kernel_spmd(nc, in_maps, core_ids=list(range(8)))
    R = res.results
    g = lambda n: [np.asarray(R[c][n]) for c in range(8)]
    y_prompt = np.stack(g("yp"), 0)
    y_sample = np.concatenate([a.reshape(16, 4, D) for a in g("ys")], 0)
    mk = np.stack(g("mk"), 1).reshape(DEPTH, 8, NMEM, 4, 256)
    mv = np.stack(g("mv"), 1).reshape(DEPTH, 8, NMEM, 4, 256)
    Cp = np.stack(g("Cp"), 1)
    np_ = np.stack(g("np_"), 1)
    mp = np.stack(g("mp"), 1)
    Cs = np.concatenate(g("Cs"), 1)
    ns = np.concatenate([a.reshape(2, 16, 4, 256) for a in g("ns")], 1)
    ms = np.concatenate(g("ms"), 1)
    pbp = np.stack(g("pbp"), 1)
    pbs = np.concatenate(g("pbs"), 1)
    return (y_prompt, y_sample, mk, mv, Cp, np_, mp, Cs, ns, ms, pbp, pbs)
```

```python
import numpy as np
import concourse.bass as bass
import concourse.mybir as mybir
from concourse.bass_utils import run_bass_kernel_spmd

F32 = mybir.dt.float32
BF16 = mybir.dt.bfloat16
AF = mybir.ActivationFunctionType
ALU = mybir.AluOpType
AX = mybir.AxisListType

D = 1024
NPR = 2048
NSM = 64
NT = NPR + NSM
DEPTH = 4
DFF = 2816
NMEM = 256
EPS = 1e-6
TT = [(0, 512), (512, 512), (1024, 512), (1536, 512), (2048, 64)]
NSLOT = 9
SLOT_EL = 2048
ARENA_EL = 26112

FLAGS = {"ffn": True, "mix": True, "xattn": True, "depth": DEPTH}


import sys as _sys


def _ln():
    f = _sys._getframe(2)
    out = []
    while f is not None and len(out) < 4:
        if f.f_code.co_name not in ("op", "dma", "_wait", "mm", "tr", "wload"):
            out.append(f.f_lineno)
        f = f.f_back
    return out


class Dep:
    __slots__ = ("w", "r", "dead")

    def __init__(self):
        self.w = None
        self.r = []
        self.dead = False


class Eng:
    def __init__(self, nc, h, name):
        self.h = h
        self.name = name
        self.sem = nc.alloc_semaphore("prog_" + name)
        self.cnt = 0
        self.waited = {}
        self.log = []


class K:
    def __init__(self, nc):
        self.nc = nc
        self.pe = Eng(nc, nc.tensor, "pe")
        self.act = Eng(nc, nc.scalar, "act")
        self.dve = Eng(nc, nc.vector, "dve")
        self.pool = Eng(nc, nc.gpsimd, "pool")
        self.sp = Eng(nc, nc.sync, "sp")
        self.sems = {}
        for e in (self.pe, self.act, self.dve, self.pool, self.sp):
            self.sems[e.sem.num] = e.sem
        self.dsem = {}
        for qn in ("pool", "sp", "act"):
            lst = []
            for i in range(12):
                s = nc.alloc_semaphore("d_%s_%d" % (qn, i))
                self.sems[s.num] = s
                lst.append([s, 0])
            self.dsem[qn] = [lst, 0]
        self.ps = nc.alloc_psum_tensor("psall", [128, 8, 512], F32)
        self.psd = [Dep() for _ in range(8)]
        self.psi = 0
        self.ring = nc.alloc_sbuf_tensor("ring", [128, NSLOT, SLOT_EL], BF16)
        self.ringd = [Dep() for _ in range(NSLOT)]
        self.ringi = 0

    def _wait(self, E, toks):
        need = {}
        for t in toks:
            if t is None:
                continue
            s, v = t
            if need.get(s, 0) < v:
                need[s] = v
        for s, v in need.items():
            if s == E.sem.num and v > E.cnt:
                continue
            if E.waited.get(s, 0) < v:
                E.h.wait_ge(self.sems[s], v)
                E.waited[s] = v
                E.log.append(("w", s, v, _ln()))

    def _deps(self, R, W):
        toks = []
        for d in R:
            assert not d.dead, "use of a recycled ring slot / psum bank"
            toks.append(d.w)
        for d in W:
            assert not d.dead, "use of a recycled ring slot"
            toks.append(d.w)
            toks.extend(d.r)
        return toks

    def op(self, E, fn, R=(), W=(), inc=True):
        self._wait(E, self._deps(R, W))
        ins = fn()
        if inc:
            E.cnt += 1
            ins.then_inc(E.sem, 1)
            E.log.append(("i", E.sem.num, 1, _ln()))
            tok = (E.sem.num, E.cnt)
        else:
            tok = (E.sem.num, E.cnt + 1)
        for d in R:
            d.r.append(tok)
        for d in W:
            d.w = tok
            d.r = []
        return ins

    def dma(self, E, out, in_, R=(), W=(), **kw):
        pool = self.dsem[E.name]
        lst, i = pool
        ent = lst[i % len(lst)]
        pool[1] = i + 1
        s, c = ent
        toks = self._deps(R, W)
        toks.append((s.num, c))
        self._wait(E, toks)
        ent[1] = c + 16
        E.h.dma_start(out=out, in_=in_, **kw).then_inc(s, 16)
        E.log.append(("i", s.num, 16, _ln()))
        tok = (s.num, c + 16)
        for d in R:
            d.r.append(tok)
        for d in W:
            d.w = tok
            d.r = []

    def finish(self):
        toks = []
        for qn, (lst, _) in self.dsem.items():
            for s, c in lst:
                if c:
                    toks.append((s.num, c))
        for e in (self.pe, self.act, self.dve, self.pool):
            if e.cnt:
                toks.append((e.sem.num, e.cnt))
        self._wait(self.sp, toks)
        self.check_deadlock()

    def check_deadlock(self):
        engs = [self.pe, self.act, self.dve, self.pool, self.sp]
        pos = [0] * len(engs)
        val = {}
        prog = True
        while prog:
            prog = False
            for i, e in enumerate(engs):
                while pos[i] < len(e.log):
                    kind, s_, v, _l = e.log[pos[i]]
                    if kind == "w":
                        if val.get(s_, 0) < v:
                            break
                    else:
                        val[s_] = val.get(s_, 0) + v
                    pos[i] += 1
                    prog = True
        bad = [(e.name, pos[i], len(e.log), e.log[pos[i]], self.sems[e.log[pos[i]][1]].name) for i, e in enumerate(engs) if pos[i] < len(e.log)]
        assert not bad, "DEADLOCK: %s" % (bad,)

    NROT = 6

    def psum_pin(self, j):
        i = self.NROT + j
        return self.ps[:, i, :], [self.psd[i]]

    def _renew(self, i):
        old = self.psd[i]
        new = Dep()
        new.r = list(old.r) + ([old.w] if old.w is not None else [])
        old.dead = True
        self.psd[i] = new

    def psum(self, n=1):
        if n == 2 and self.psi % 2:
            self.psi += 1
        i = self.psi % self.NROT
        self.psi += n
        for q in range(n):
            self._renew(i + q)
        if n == 1:
            return self.ps[:, i, :], [self.psd[i]]
        return self.ps[:, i:i + 2, :], [self.psd[i], self.psd[i + 1]]

    def slot(self):
        i = self.ringi % NSLOT
        self.ringi += 1
        old = self.ringd[i]
        new = Dep()
        new.r = list(old.r) + ([old.w] if old.w is not None else [])
        old.dead = True
        self.ringd[i] = new
        return self.ring[:, i, :], new

    def mm(self, out, lhsT, rhs, start, stop, R, W, inc):
        return self.op(self.pe, lambda: self.nc.tensor.matmul(out, lhsT=lhsT, rhs=rhs, start=start, stop=stop),
                       R, W, inc)

    def tr(self, out, in_, ident, R, W, inc):
        return self.op(self.pe, lambda: self.nc.tensor.transpose(out, in_, ident), R, W, inc)

    def wload(self, src_ap, shape):
        ap, d = self.slot()
        n = 1
        for s in shape[1:]:
            n *= s
        assert n <= SLOT_EL, shape
        v = ap[0:shape[0], 0:n]
        if len(shape) == 3:
            v = v.rearrange("p (a b) -> p a b", a=shape[1])
        self.dma(self.pool, v, src_ap, R=(), W=(d,))
        return v, d


def rows_kc(w_ap, c0, c1):
    return w_ap.rearrange("(kc p) n -> p kc n", p=128)[:, :, c0:c1]


def build():
    nc = bass.Bass("TRN2", target_bir_lowering=False)
    k = K(nc)
    PE, ACT, DVE, POOL, SP = k.pe, k.act, k.dve, k.pool, k.sp
    V = nc.vector
    A = nc.scalar
    G = nc.gpsimd
    nd = FLAGS["depth"]

    def din(name, shape):
        return nc.dram_tensor(name, list(shape), F32, kind="ExternalInput").ap()

    def dout(name, shape):
        return nc.dram_tensor(name, list(shape), F32, kind="ExternalOutput").ap()

    xp_d = din("xp", [NPR, D]); xs_d = din("xs", [NSM, D]); mem_d = din("mem", [NMEM, D])
    ck_d = din("ck", [DEPTH, 16, NMEM, D]); cv_d = din("cv", [DEPTH, 16, NMEM, D])
    sC_d = din("sC", [2, 16, 4, 256, 256]); sn_d = din("sn", [2, 64, 256]); sm_d = din("sm", [2, 16, 4])
    spb_d = din("spb", [2, 240, D])
    vecs_d = din("vecs", [256, 128]); gb_d = din("gb", [4, 4]); cst_d = din("cst", [128, 512])
    rm_d = din("rm", [4, NT])
    w_up_d = din("ffn_w_up", [DEPTH, 2, D, 2 * DFF]); w_dn_d = din("ffn_w_down", [DEPTH, 2, DFF, D])
    mw_in_d = din("mlstm_w_in", [2, D, 4104]); mw_out_d = din("mlstm_w_out", [2, D, D])
    pw_in_d = din("pool_w_in", [2, D, D]); pw_grp_d = din("pool_w_grp", [2, 4, 256, 256]); pw_out_d = din("pool_w_out", [2, D, D])
    xw_q_d = din("xattn_w_q", [DEPTH, D, D]); xw_kv_d = din("xattn_w_kv", [DEPTH, D, 2 * D]); xw_o_d = din("xattn_w_o", [DEPTH, D, D])

    yp_d = dout("yp", [NPR, D]); ys_d = dout("ys", [NSM, D])
    mk_d = dout("mk", [DEPTH, NMEM, D]); mv_d = dout("mv", [DEPTH, NMEM, D])
    Cp_d = dout("Cp", [2, 4, 256, 256]); np_d = dout("np_", [2, 4, 256]); mp_d = dout("mp", [2, 4])
    Cs_d = dout("Cs", [2, 16, 4, 256, 256]); ns_d = dout("ns", [2, 64, 256]); ms_d = dout("ms", [2, 16, 4])
    pbp_d = dout("pbp", [2, 15, D]); pbs_d = dout("pbs", [2, 16, 15, D])

    xres = nc.alloc_sbuf_tensor("xres", [128, 8, NT], F32)
    xres_d = [[Dep() for _ in TT] for _ in range(8)]
    xn = nc.alloc_sbuf_tensor("xn", [128, 8, NT], BF16)
    xn_d = [Dep() for _ in TT]
    arena = nc.alloc_sbuf_tensor("arena", [128, ARENA_EL], BF16)
    cst = nc.alloc_sbuf_tensor("cst_sb", [128, 512], F32)
    cst_dep = Dep()
    identb = nc.alloc_sbuf_tensor("identb", [128, 128], BF16)
    Ub = nc.alloc_sbuf_tensor("Ub", [128, 128], BF16)
    maskbd = nc.alloc_sbuf_tensor("maskbd", [64, 64], BF16)
    onesb = nc.alloc_sbuf_tensor("onesb", [128, 128], BF16)
    rmb = nc.alloc_sbuf_tensor("rmb", [64, 16], BF16)
    ones4 = nc.alloc_sbuf_tensor("ones4", [4, 128], F32)
    VEC = nc.alloc_sbuf_tensor("VEC", [128, 256], F32)
    gb = nc.alloc_sbuf_tensor("gb_sb", [4, 6], F32)
    const_dep = Dep()
    rstd = nc.alloc_sbuf_tensor("rstd", [128, 1, 512], F32)
    rstd_d = [Dep(), Dep()]
    sqbuf = nc.alloc_sbuf_tensor("sqbuf", [128, 4, 512], BF16)
    sqbuf_d = Dep()
    ident = cst[:, 0:128]
    U32 = cst[:, 128:256]

    k.dma(SP, cst[:], cst_d, W=(cst_dep,))
    k.dma(SP, gb[:, 0:4], gb_d, W=(const_dep,))
    k.op(DVE, lambda: V.tensor_copy(out=identb[:], in_=cst[:, 0:128]), R=(cst_dep,), W=(const_dep,))
    k.op(DVE, lambda: V.tensor_copy(out=Ub[:], in_=cst[:, 128:256]), R=(cst_dep,), W=(const_dep,))
    k.op(DVE, lambda: V.tensor_copy(out=maskbd[:], in_=cst[0:64, 256:320]), R=(cst_dep,), W=(const_dep,))
    k.op(DVE, lambda: V.memset(onesb[:], 1.0), W=(const_dep,))
    k.op(DVE, lambda: V.tensor_copy(out=rmb[:], in_=cst[0:64, 320:336]), R=(cst_dep,), W=(const_dep,))
    k.op(DVE, lambda: V.memset(ones4[:], 1.0), W=(const_dep,))
    k.op(DVE, lambda: V.tensor_scalar_mul(out=gb[:, 4:6], in0=gb[:, 2:4], scalar1=-1.0), R=(const_dep,), W=(const_dep,))
    vtmp, vtd = k.slot()
    vt32 = vtmp.bitcast(F32)
    k.dma(SP, vt32[:, 0:256].rearrange("p (a b) -> p a b", a=2), vecs_d.rearrange("(a p) n -> p a n", p=128), W=(vtd,))
    pv, pvd = k.psum()
    for a in range(2):
        k.tr(pv[:, a * 128:(a + 1) * 128], vt32[:, a * 128:(a + 1) * 128], ident, R=(vtd, cst_dep), W=pvd, inc=(a == 1))
    k.op(DVE, lambda: V.tensor_copy(out=VEC[:], in_=pv[:, 0:256]), R=pvd, W=(const_dep,))

    if FLAGS.get("stop") == 1:
        k.finish(); return nc

    def ng_col(l, i, c):
        j = (l * 4 + i) * 8 + c
        return VEC[:, j:j + 1]

    def load_transpose(src_rows_ap, nrows, col0):
        st, sd = k.slot()
        st2, sd2 = k.slot()
        s32a = st.bitcast(F32)
        s32b = st2.bitcast(F32)
        k.dma(SP, s32a[0:nrows, 0:512], src_rows_ap[:, 0:512], W=(sd,))
        k.dma(SP, s32b[0:nrows, 0:512], src_rows_ap[:, 512:1024], W=(sd2,))
        for hlf, (s32, dd) in enumerate(((s32a, sd), (s32b, sd2))):
            pt, ptd = k.psum()
            for c4 in range(4):
                k.tr(pt[:, c4 * 128:c4 * 128 + nrows], s32[0:nrows, c4 * 128:(c4 + 1) * 128], ident[0:nrows, 0:nrows],
                     R=(dd, cst_dep), W=ptd, inc=(c4 == 3))
            tti = min(col0 // 512, 4)
            wd = [xres_d[hlf * 4 + c4][tti] for c4 in range(4)]
            src = pt.rearrange("p (c t) -> p c t", c=4)[:, :, 0:nrows]
            eng = ACT if hlf == 0 else DVE
            if eng is ACT:
                k.op(ACT, lambda: A.copy(out=xres[:, 0:4, col0:col0 + nrows], in_=src), R=ptd, W=wd)
            else:
                k.op(DVE, lambda: V.tensor_copy(out=xres[:, 4:8, col0:col0 + nrows], in_=src), R=ptd, W=wd)

    for t in range(16):
        load_transpose(xp_d[t * 128:(t + 1) * 128, :], 128, t * 128)
    load_transpose(xs_d, 64, NPR)

    if FLAGS.get("stop") == 2:
        k.finish(); return nc
    def rmsnorm_part1(tti):
        t0, n = TT[tti]
        pr, prd = k.psum()
        for hlf in range(2):
            sqv, sqd = sqbuf, sqbuf_d
            k.op(ACT, lambda: A.activation(out=sqv[:, :, 0:n], in_=xres[:, hlf * 4:hlf * 4 + 4, t0:t0 + n], func=AF.Square),
                 R=[xres_d[hlf * 4 + c][tti] for c in range(4)], W=(sqd,))
            for c in range(4):
                k.mm(pr[:, 0:n], onesb[:], sqv[:, c, 0:n], start=(hlf == 0 and c == 0), stop=(hlf == 1 and c == 3),
                     R=(sqd, const_dep), W=prd, inc=(c == 3))
        ri = 0
        rs = rstd[:, ri, 0:n]
        k.op(ACT, lambda: A.activation(out=rs, in_=pr[:, 0:n], func=AF.Sqrt, scale=1.0 / D, bias=cst[:, 448:449]),
             R=prd + [cst_dep], W=(rstd_d[ri],))

    def rmsnorm_part2(tti, gcol_fn, out_fn):
        t0, n = TT[tti]
        ri = 0
        rs = rstd[:, ri, 0:n]
        k.op(DVE, lambda: V.reciprocal(out=rs, in_=rs), R=(), W=(rstd_d[ri],))
        for c in range(8):
            oap, odeps = out_fn(c)
            k.op(DVE, lambda: V.scalar_tensor_tensor(out=oap, in0=xres[:, c, t0:t0 + n], scalar=gcol_fn(c), in1=rs,
                                                     op0=ALU.mult, op1=ALU.mult),
                 R=(xres_d[c][tti], rstd_d[ri], const_dep), W=odeps)

    def rmsnorm_tile(tti, gcol_fn, out_fn, out_deps_fn):
        rmsnorm_part1(tti)
        rmsnorm_part2(tti, gcol_fn, out_fn)

    norm_state = {"done": set(), "next": None, "pending": None}

    def flush_pending():
        p = norm_state["pending"]
        if p is not None:
            l_, i_, t_ = p
            t0, n = TT[t_]
            rmsnorm_part2(t_, lambda c: ng_col(l_, i_, c), lambda c: (xn[:, c, t0:t0 + n], (xn_d[t_],)))
            norm_state["done"].add(p)
            norm_state["pending"] = None

    def norm_tile_xn(l, i, tti):
        t0, n = TT[tti]
        rmsnorm_tile(tti, lambda c: ng_col(l, i, c), lambda c: (xn[:, c, t0:t0 + n], (xn_d[tti],)), None)

    def norm_to_xn(l, i):
        flush_pending()
        for tti in range(len(TT)):
            if (l, i, tti) not in norm_state["done"]:
                norm_tile_xn(l, i, tti)

    def early_norm(tti):
        nx = norm_state["next"]
        if nx is not None and FLAGS.get("early_norm", True):
            flush_pending()
            rmsnorm_part1(tti)
            norm_state["pending"] = (nx[0], nx[1], tti)

    arena_live = []

    def arena_deps(shape):
        toks = []
        for d in arena_live:
            if d.w is not None:
                toks.append(d.w)
            toks.extend(d.r)
        dd = {}
        for t in toks:
            if dd.get(t[0], 0) < t[1]:
                dd[t[0]] = t[1]
        toks = list(dd.items())
        del arena_live[:]

        def mk(sh):
            if not sh:
                d = Dep()
                d.r = list(toks)
                arena_live.append(d)
                return d
            return [mk(sh[1:]) for _ in range(sh[0])]
        return mk(list(shape))

    act = arena[:, 0:12 * NT].rearrange("p (c t) -> p c t", c=12)

    def ffn(l, i):
        act_d = arena_deps([12, len(TT)])
        norm_to_xn(l, 0 if i == 0 else 3)
        wu = w_up_d[l, i]
        wd_ = w_dn_d[l, i]
        for half, units in enumerate((range(0, 6), range(6, 11))):
            nchunk = len(units) * 2
            for ui, u in enumerate(units):
                Gp, Gd = k.wload(rows_kc(wu, u * 256, (u + 1) * 256), [128, 8, 256])
                Up, Ud = k.wload(rows_kc(wu, DFF + u * 256, DFF + (u + 1) * 256), [128, 8, 256])
                for tti, (t0, n) in enumerate(TT):
                    for jj in range(2):
                        cl = ui * 2 + jj
                        pg, pgd = k.psum()
                        for kc in range(8):
                            k.mm(pg[:, 0:n], Gp[:, kc, jj * 128:(jj + 1) * 128], xn[:, kc, t0:t0 + n], kc == 0, kc == 7,
                                 R=(Gd, xn_d[tti]), W=pgd, inc=(kc == 7))
                        pu, pud = k.psum()
                        for kc in range(8):
                            k.mm(pu[:, 0:n], Up[:, kc, jj * 128:(jj + 1) * 128], xn[:, kc, t0:t0 + n], kc == 0, kc == 7,
                                 R=(Ud, xn_d[tti]), W=pud, inc=(kc == 7))
                        k.op(ACT, lambda: A.activation(out=act[:, cl, t0:t0 + n], in_=pg[:, 0:n], func=AF.Silu), R=pgd, W=(act_d[cl][tti],))
                        k.op(DVE, lambda: V.tensor_tensor(out=act[:, cl, t0:t0 + n], in0=pu[:, 0:n], in1=act[:, cl, t0:t0 + n], op=ALU.mult),
                             R=pud, W=(act_d[cl][tti],))
            Dps = []
            for ui, u in enumerate(units):
                Dp, Dd = k.wload(wd_[u * 256:(u + 1) * 256, :].rearrange("(a p) n -> p a n", p=128), [128, 2, 1024])
                Dps.append((Dp, Dd))
            for tti, (t0, n) in enumerate(TT):
                for m in range(8):
                    po, pod = k.psum()
                    for cl in range(nchunk):
                        Dp, Dd = Dps[cl // 2]
                        k.mm(po[:, 0:n], Dp[:, cl % 2, m * 128:(m + 1) * 128], act[:, cl, t0:t0 + n], cl == 0, cl == nchunk - 1,
                             R=(Dd, act_d[cl][tti]), W=pod, inc=(cl == nchunk - 1))
                    k.op(DVE, lambda: V.scalar_tensor_tensor(out=xres[:, m, t0:t0 + n], in0=po[:, 0:n], scalar=0.5,
                                                             in1=xres[:, m, t0:t0 + n], op0=ALU.mult, op1=ALU.add),
                         R=pod, W=(xres_d[m][tti],))
                if half == 1:
                    early_norm(tti)

    memT = nc.alloc_sbuf_tensor("memT", [128, 8, NMEM], BF16)
    memT_d = Dep()
    for mc in range(2):
        for hlf in range(2):
            st, sd = k.slot()
            s32 = st.bitcast(F32)
            k.dma(SP, s32[:, 0:512], mem_d[mc * 128:(mc + 1) * 128, hlf * 512:(hlf + 1) * 512], W=(sd,))
            pt, ptd = k.psum()
            for c4 in range(4):
                k.tr(pt[:, c4 * 128:(c4 + 1) * 128], s32[:, c4 * 128:(c4 + 1) * 128], ident, R=(sd, cst_dep), W=ptd, inc=(c4 == 3))
            k.op(ACT, lambda: A.copy(out=memT[:, hlf * 4:hlf * 4 + 4, mc * 128:(mc + 1) * 128],
                                     in_=pt.rearrange("p (c t) -> p c t", c=4)), R=ptd, W=(memT_d,))

    def xattn(l):
        QO = 8 * NT
        qT = arena[:, 0:QO].rearrange("p (c t) -> p c t", c=8)
        KT = arena[:, QO:QO + 2048].rearrange("p (c m) -> p c m", c=8)
        Vp = arena[:, QO + 2048:QO + 4096].rearrange("p (c m) -> p c m", c=2)
        rsS = arena[:, QO + 4096:QO + 4096 + 512].bitcast(F32)
        dl = arena_deps([12])
        qT_d, KT_d, Vp_d, rsS_d = dl[0:8], dl[8], dl[9], dl[10]
        qT_td = [[Dep() for _ in TT] for _ in range(8)]
        for c in range(8):
            for tti in range(len(TT)):
                qT_td[c][tti].r = list(qT_d[c].r)
                arena_live.append(qT_td[c][tti])
        norm_to_xn(l, 2)
        wkv = xw_kv_d[l]
        if FLAGS.get("xstop") == 0:
            return
        for pi in range(8):
            Wp, Wd = k.wload(rows_kc(wkv, pi * 256, (pi + 1) * 256), [128, 8, 256])
            h = pi % 4
            if pi < 4:
                for dc in range(2):
                    ps, psd = k.psum()
                    for kc in range(8):
                        k.mm(ps[:, 0:256], Wp[:, kc, dc * 128:(dc + 1) * 128], memT[:, kc, :], kc == 0, kc == 7,
                             R=(Wd, memT_d), W=psd, inc=(kc == 7))
                    k.op(ACT, lambda: A.copy(out=KT[:, h * 2 + dc, :], in_=ps[:, 0:256]), R=psd, W=(KT_d,))
            for mc in range(2):
                ps, psd = k.psum()
                for kc in range(8):
                    k.mm(ps[:, 0:256], memT[:, kc, mc * 128:(mc + 1) * 128], Wp[:, kc, :], kc == 0, kc == 7,
                         R=(Wd, memT_d), W=psd, inc=(kc == 7))
                if True:
                    stg, stgd = k.slot()
                    st32 = stg.bitcast(F32)[:, 0:256]
                    k.op(DVE, lambda: V.tensor_copy(out=st32, in_=ps[:, 0:256]), R=psd, W=(stgd,))
                    if pi >= 4:
                        k.op(ACT, lambda: A.copy(out=Vp[:, mc, h * 256:(h + 1) * 256], in_=st32), R=(stgd,), W=(Vp_d,))
                    dst = (mk_d if pi < 4 else mv_d)[l, mc * 128:(mc + 1) * 128, h * 256:(h + 1) * 256]
                    k.dma(SP, dst, st32, R=(stgd,))
        if FLAGS.get("xstop") == 1:
            return
        wq = xw_q_d[l]
        for pi in range(4):
            Wp, Wd = k.wload(rows_kc(wq, pi * 256, (pi + 1) * 256), [128, 8, 256])
            for tti, (t0, n) in enumerate(TT):
                for oc in range(2):
                    c = pi * 2 + oc
                    ps, psd = k.psum()
                    for kc in range(8):
                        k.mm(ps[:, 0:n], Wp[:, kc, oc * 128:(oc + 1) * 128], xn[:, kc, t0:t0 + n], kc == 0, kc == 7,
                             R=(Wd, xn_d[tti]), W=psd, inc=(kc == 7))
                    k.op(ACT, lambda: A.mul(out=qT[:, c, t0:t0 + n], in_=ps[:, 0:n], mul=1.0 / 16.0), R=psd, W=(qT_td[c][tti],))
        if FLAGS.get("xstop") == 2:
            return
        oT = xn
        def prompt_s1(h, tti):
            t0, n = TT[tti]
            est, esd = k.slot()
            eT = est[:, 0:1024].rearrange("p (c t) -> p c t", c=2)
            for mc in range(2):
                ps, psd = k.psum()
                for dc in range(2):
                    k.mm(ps[:, :], KT[:, h * 2 + dc, mc * 128:(mc + 1) * 128], qT[:, h * 2 + dc, t0:t0 + n], dc == 0, dc == 1,
                         R=(KT_d, qT_td[h * 2 + dc][tti]), W=psd, inc=(dc == 1))
                k.op(ACT, lambda: A.activation(out=eT[:, mc, :], in_=ps[:, :], func=AF.Exp), R=psd, W=(esd,))
            return eT, esd

        def prompt_s2(h, tti, st1):
            eT, esd = st1
            t0, n = TT[tti]
            pss, pssd = k.psum()
            for mc in range(2):
                k.mm(pss[:, :], onesb[:], eT[:, mc, :], mc == 0, mc == 1, R=(esd, const_dep), W=pssd, inc=(mc == 1))
            rst, rsd = k.slot()
            rs32 = rst.bitcast(F32)[:, 0:512]
            k.op(DVE, lambda: V.reciprocal(out=rs32, in_=pss[:, :]), R=pssd, W=(rsd,))
            for dc in range(2):
                ps, psd = k.psum()
                for mc in range(2):
                    k.mm(ps[:, :], Vp[:, mc, h * 256 + dc * 128:h * 256 + (dc + 1) * 128], eT[:, mc, :], mc == 0, mc == 1,
                         R=(Vp_d, esd), W=psd, inc=(mc == 1))
                k.op(DVE, lambda: V.tensor_tensor(out=oT[:, h * 2 + dc, t0:t0 + n], in0=ps[:, :], in1=rs32, op=ALU.mult),
                     R=psd + [rsd], W=(xn_d[tti],))
        psS, psSd = k.psum_pin(0)

        def sample_a(b):
            KTb, KTbd = k.slot()
            KTv = KTb.rearrange("p (c m) -> p c m", c=8)
            Kb, Kbd = k.wload(ck_d[l, b].rearrange("(mc p) d -> p mc d", p=128), [128, 2, 1024])
            for half in range(2):
                pt, ptd = k.psum()
                ptb = pt.bitcast(BF16)
                for q4 in range(4):
                    hd = half * 4 + q4
                    for mc in range(2):
                        k.tr(ptb[:, q4 * 256 + mc * 128:q4 * 256 + (mc + 1) * 128], Kb[:, mc, hd * 128:(hd + 1) * 128], identb[:],
                             R=(Kbd, const_dep), W=ptd, inc=(q4 == 3 and mc == 1))
                if half == 0:
                    k.op(ACT, lambda: A.copy(out=KTb[:, 0:1024], in_=ptb[:, 0:1024]), R=ptd, W=(KTbd,))
                else:
                    k.op(DVE, lambda: V.tensor_copy(out=KTb[:, 1024:2048], in_=ptb[:, 0:1024]), R=ptd, W=(KTbd,))
            for h in range(4):
                for mc in range(2):
                    col = b * 32 + (h * 2 + mc) * 4
                    for dc in range(2):
                        k.mm(psS[:, col:col + 4], KTv[:, h * 2 + dc, mc * 128:(mc + 1) * 128],
                             qT[:, h * 2 + dc, NPR + 4 * b:NPR + 4 * b + 4], dc == 0, dc == 1,
                             R=(KTbd, qT_td[h * 2 + dc][4]), W=psSd, inc=(h == 3 and mc == 1 and dc == 1))
        st1_next = prompt_s1(0, 0)
        for idx in range(16):
            st1_cur = st1_next
            if idx + 1 < 16:
                st1_next = prompt_s1((idx + 1) // 4, (idx + 1) % 4)
            sample_a(idx)
            prompt_s2(idx // 4, idx % 4, st1_cur)
        eS = arena[:, QO + 5120:QO + 5120 + 512]
        eSd = dl[11]
        k.op(ACT, lambda: A.activation(out=eS, in_=psS[:, :], func=AF.Exp), R=psSd, W=(eSd,))
        eS5 = eS.rearrange("p (b h mc t) -> p b h mc t", b=16, h=4, mc=2)
        pss, pssd = k.psum()
        for b in range(16):
            for mc in range(2):
                k.mm(pss[:, b * 16:(b + 1) * 16], onesb[:], eS5[:, b, :, mc, :], mc == 0, mc == 1, R=(eSd, const_dep), W=pssd,
                     inc=(b == 15 and mc == 1))
        k.op(DVE, lambda: V.reciprocal(out=rsS[:, :], in_=pss[:, 0:256]), R=pssd, W=(rsS_d,))
        if FLAGS.get("xstop") == 4:
            return
        wo = xw_o_d[l]

        def wo_tiles(tiles):
            Wps = [k.wload(rows_kc(wo, pi * 256, (pi + 1) * 256), [128, 8, 256]) for pi in range(4)]
            for tti in tiles:
                t0, n = TT[tti]
                for m in range(8):
                    Wp, Wd = Wps[m // 2]
                    oc = m % 2
                    ps, psd = k.psum()
                    for kc in range(8):
                        k.mm(ps[:, 0:n], Wp[:, kc, oc * 128:(oc + 1) * 128], oT[:, kc, t0:t0 + n], kc == 0, kc == 7,
                             R=(Wd, xn_d[tti]), W=psd, inc=(kc == 7))
                    k.op(DVE, lambda: V.tensor_tensor(out=xres[:, m, t0:t0 + n], in0=ps[:, 0:n], in1=xres[:, m, t0:t0 + n], op=ALU.add),
                         R=psd, W=(xres_d[m][tti],))
                early_norm(tti)

        wo_tiles([0, 1, 2, 3])
        psO, psOd = k.psum_pin(1)
        psO4 = psO.rearrange("p (hd b t) -> p hd b t", hd=8, b=16)
        for b in range(16):
            Vb, Vbd = k.wload(cv_d[l, b].rearrange("(mc p) d -> p mc d", p=128), [128, 2, 1024])
            for hd in range(8):
                h = hd // 2
                for mc in range(2):
                    k.mm(psO4[:, hd, b, :], Vb[:, mc, hd * 128:(hd + 1) * 128], eS5[:, b, h, mc, :], mc == 0, mc == 1,
                         R=(Vbd, eSd), W=psOd, inc=(hd == 7 and mc == 1))
        rs4 = rsS.rearrange("p (b h t) -> p h b t", b=16, h=4)
        for hd in range(8):
            o_ap = oT[:, hd, NPR:NT].rearrange("p (b t) -> p b t", b=16)
            i_ap = psO4[:, hd, :, :]
            k.op(DVE, lambda: V.tensor_tensor(out=o_ap, in0=i_ap, in1=rs4[:, hd // 2, :, :], op=ALU.mult), R=psOd + [rsS_d], W=(xn_d[4],))
        wo_tiles([4])

    tmp16 = nc.alloc_sbuf_tensor("tmp16", [128, 2, 16], F32)
    tmp16_d = Dep()

    def pool_mixer(l):
        j = l // 2
        PW = 15 + NPR
        FW = PW + 16 * 19
        bufs = []
        for x in range(2):
            off = x * 2 * FW * 2
            t32 = arena[:, off:off + 2 * FW * 2].bitcast(F32).rearrange("p (c w) -> p c w", c=2)
            bufs.append((t32[:, :, 0:PW], t32[:, :, PW:FW].rearrange("p c (b i) -> p c b i", b=16)))
        uoff = 2 * 2 * FW * 2
        ub = arena[:, uoff:uoff + 2 * NT].rearrange("p (c t) -> p c t", c=2)
        woff = uoff + 2 * NT
        dl = arena_deps([5])
        AB_d = [dl[0], dl[1]]
        ub_d = dl[2]
        Wgd, Wod = dl[3], dl[4]
        norm_to_xn(l, 1)
        k.dma(SP, pbs_d[j, :, 0:11, :], spb_d[j].rearrange("(b i) d -> b i d", i=15)[:, 4:15, :])
        for g in range(4):
            w = 2 << g
            Win, Wind = k.wload(rows_kc(pw_in_d[j], g * 256, (g + 1) * 256), [128, 8, 256])
            Wg = arena[:, woff:woff + 512].rearrange("p (a n) -> p a n", a=2)
            Wo = arena[:, woff + 512:woff + 2560].rearrange("p (a n) -> p a n", a=2)
            k.dma(POOL, Wg, pw_grp_d[j, g].rearrange("(a p) n -> p a n", p=128), W=(Wgd,))
            k.dma(POOL, Wo, pw_out_d[j][g * 256:(g + 1) * 256, :].rearrange("(a p) n -> p a n", p=128), W=(Wod,))
            (Ap, As), (Bp, Bs) = bufs
            k.op(DVE, lambda: V.memset(Ap[:, :, 0:15], 0.0), W=(AB_d[0],))
            k.op(DVE, lambda: V.memset(Bp[:, :, 0:15], 0.0), W=(AB_d[1],))
            for cc in range(2):
                pt, ptd = k.psum()
                for rt, nr in ((0, 128), (1, 112)):
                    st, sd = k.slot()
                    s32 = st.bitcast(F32)
                    k.dma(SP, s32[0:nr, 0:128], spb_d[j, rt * 128:rt * 128 + nr, g * 256 + cc * 128:g * 256 + (cc + 1) * 128], W=(sd,))
                    k.tr(pt[:, rt * 128:rt * 128 + nr], s32[0:nr, 0:128], ident[0:nr, 0:nr], R=(sd, cst_dep), W=ptd, inc=(rt == 1))
                k.op(ACT, lambda: A.copy(out=As[:, cc, :, 0:15], in_=pt[:, 0:240].rearrange("p (b i) -> p b i", b=16)),
                     R=ptd, W=(AB_d[0],))
            for tti, (t0, n) in enumerate(TT):
                for cc in range(2):
                    ps, psd = k.psum()
                    for kc in range(8):
                        k.mm(ps[:, 0:n], Win[:, kc, cc * 128:(cc + 1) * 128], xn[:, kc, t0:t0 + n], kc == 0, kc == 7,
                             R=(Wind, xn_d[tti]), W=psd, inc=(kc == 7))
                    if tti < 4:
                        k.op(ACT, lambda: A.copy(out=Ap[:, cc, 15 + t0:15 + t0 + n], in_=ps[:, 0:n]), R=psd, W=(AB_d[0],))
                        k.op(DVE, lambda: V.tensor_copy(out=ub[:, cc, t0:t0 + n], in_=Ap[:, cc, 15 + t0:15 + t0 + n]),
                             R=(AB_d[0],), W=(ub_d,))
                    else:
                        k.op(ACT, lambda: A.copy(out=As[:, cc, :, 15:19], in_=ps[:, 0:64].rearrange("p (b t) -> p b t", b=16)),
                             R=psd, W=(AB_d[0],))
                        k.op(DVE, lambda: V.tensor_copy(out=ub[:, cc, NPR:NT].rearrange("p (b t) -> p b t", b=16), in_=As[:, cc, :, 15:19]),
                             R=(AB_d[0],), W=(ub_d,))
            for (c0, mrows, which) in ((NPR - 15, 15, 0), (NPR, 64, 1)):
                ps, psd = k.psum()
                for kc in range(8):
                    k.mm(ps[0:mrows, 0:256], xn[:, kc, c0:c0 + mrows], Win[:, kc, :], kc == 0, kc == 7,
                         R=(Wind, xn_d[3 if which == 0 else 4]), W=psd, inc=(kc == 7))
                stg, stgd = k.slot()
                st32 = stg.bitcast(F32)
                k.op(DVE, lambda: V.tensor_copy(out=st32[0:mrows, 0:256], in_=ps[0:mrows, 0:256]), R=psd, W=(stgd,))
                if which == 0:
                    k.dma(SP, pbp_d[j, :, g * 256:(g + 1) * 256], st32[0:15, 0:256], R=(stgd,))
                else:
                    for b in range(16):
                        k.dma(SP, pbs_d[j, b, 11:15, g * 256:(g + 1) * 256], st32[4 * b:4 * b + 4, 0:256], R=(stgd,))
            cur = 0
            for lev in range(g + 1):
                sh = 1 << lev
                (ip, is_), (op_, os_) = bufs[cur], bufs[1 - cur]
                k.op(DVE, lambda: V.tensor_tensor(out=op_[:, :, sh:PW], in0=ip[:, :, sh:PW], in1=ip[:, :, 0:PW - sh], op=ALU.add),
                     R=(AB_d[cur],), W=(AB_d[1 - cur],))
                k.op(DVE, lambda: V.tensor_tensor(out=os_[:, :, :, sh:19], in0=is_[:, :, :, sh:19], in1=is_[:, :, :, 0:19 - sh], op=ALU.add),
                     R=(AB_d[cur],), W=(AB_d[1 - cur],))
                cur = 1 - cur
            wp, ws = bufs[cur]
            wd_ = AB_d[cur]
            invc = cst[:, 384 + g * 16:384 + (g + 1) * 16]
            k.op(DVE, lambda: V.tensor_tensor(out=tmp16[:], in0=wp[:, :, 15:31], in1=invc.unsqueeze(1).to_broadcast([128, 2, 16]), op=ALU.mult),
                 R=(wd_, cst_dep), W=(tmp16_d,))
            k.op(DVE, lambda: V.tensor_tensor(out=ub[:, :, 0:16], in0=tmp16[:], in1=ub[:, :, 0:16], op=ALU.subtract),
                 R=(tmp16_d,), W=(ub_d,))
            k.op(DVE, lambda: V.scalar_tensor_tensor(out=ub[:, :, 16:NPR], in0=wp[:, :, 31:PW], scalar=1.0 / w, in1=ub[:, :, 16:NPR],
                                                     op0=ALU.mult, op1=ALU.subtract), R=(wd_,), W=(ub_d,))
            for cc in range(2):
                us = ub[:, cc, NPR:NT].rearrange("p (b t) -> p b t", b=16)
                k.op(DVE, lambda: V.scalar_tensor_tensor(out=us, in0=ws[:, cc, :, 15:19], scalar=1.0 / w, in1=us,
                                                         op0=ALU.mult, op1=ALU.subtract), R=(wd_,), W=(ub_d,))
            for tti, (t0, n) in enumerate(TT):
                yt, ytd = k.slot()
                ytv = yt[:, 0:1024].rearrange("p (c t) -> p c t", c=2)
                for oc in range(2):
                    ps, psd = k.psum()
                    for kc2 in range(2):
                        k.mm(ps[:, 0:n], Wg[:, kc2, oc * 128:(oc + 1) * 128], ub[:, kc2, t0:t0 + n], kc2 == 0, kc2 == 1,
                             R=(Wgd, ub_d), W=psd, inc=(kc2 == 1))
                    sc = VEC[:, 136 + j * 8 + g * 2 + oc:137 + j * 8 + g * 2 + oc]
                    k.op(ACT, lambda: A.mul(out=ytv[:, oc, 0:n], in_=ps[:, 0:n], mul=sc), R=psd + [const_dep], W=(ytd,))
                for m in range(8):
                    ps, psd = k.psum()
                    for kc2 in range(2):
                        k.mm(ps[:, 0:n], Wo[:, kc2, m * 128:(m + 1) * 128], ytv[:, kc2, 0:n], kc2 == 0, kc2 == 1,
                             R=(Wod, ytd), W=psd, inc=(kc2 == 1))
                    k.op(DVE, lambda: V.tensor_tensor(out=xres[:, m, t0:t0 + n], in0=ps[:, 0:n], in1=xres[:, m, t0:t0 + n], op=ALU.add),
                         R=psd, W=(xres_d[m][tti],))
                if g == 3:
                    early_norm(tti)

    WT = nc.alloc_sbuf_tensor("WT", [128, 17, 8], F32)
    DECB = nc.alloc_sbuf_tensor("DECB", [128, 128], F32)
    Cst = nc.alloc_sbuf_tensor("Cst", [128, 2, 257], F32)
    numS = nc.alloc_sbuf_tensor("numS", [64, 257], F32)
    NST = nc.alloc_sbuf_tensor("NST", [128, 2, 64], F32)
    NSTn = nc.alloc_sbuf_tensor("NSTn", [128, 2, 64], F32)
    RR = nc.alloc_sbuf_tensor("RR", [128, 17], F32)
    SQ = nc.alloc_sbuf_tensor("SQ", [128, 17], F32)
    sml = nc.alloc_sbuf_tensor("sml", [4, 256], F32)
    DDt = nc.alloc_sbuf_tensor("DDt", [4, 128], F32)
    sptb = nc.alloc_sbuf_tensor("sptb", [128, 2, 128], BF16)
    sptb_d = [Dep(), Dep()]

    def mlstm_mixer(l):
        j = l // 2
        win = mw_in_d[j]
        dl = arena_deps([4])
        GI_d, GL_d, GB_d, RM_d = dl
        GI = arena[0:4, 0:2 * NT].bitcast(F32)
        GL = arena[0:4, 2 * NT:4 * NT].bitcast(F32)
        GB = arena[0:4, 4 * NT:6 * NT].bitcast(F32)
        RM = arena[0:4, 6 * NT:8 * NT].bitcast(F32)
        WT_d, DECB_d, Cst_d, numS_d, NST_d, NSTn_d, RR_d, SQ_d, sml_d = [Dep() for _ in range(9)]
        norm_to_xn(l, 1)
        k.dma(SP, RM, rm_d, W=(RM_d,))
        GM, BL, MM, CC, DEC, MS, MN = (sml[:, 0:32], sml[:, 32:64], sml[:, 64:97], sml[:, 100:132], sml[:, 132:164],
                                       sml[:, 164:180], sml[:, 180:196])
        k.dma(SP, MS, sm_d[j].rearrange("b h -> h b"), W=(sml_d,), allow_slow_non_contiguous=True)
        st, sd = k.slot()
        s32 = st.bitcast(F32)
        k.dma(SP, s32[0:64, 0:256], sn_d[j], W=(sd,))
        pt, ptd = k.psum()
        for dc in range(2):
            k.tr(pt[:, dc * 64:(dc + 1) * 64], s32[0:64, dc * 128:(dc + 1) * 128], ident[0:64, 0:64], R=(sd, cst_dep), W=ptd, inc=(dc == 1))
        k.op(DVE, lambda: V.tensor_copy(out=NST[:], in_=pt[:, 0:128].rearrange("p (c n) -> p c n", c=2)), R=ptd, W=(NST_d,))
        Wgt, Wgtd = k.wload(rows_kc(win, 4096, 4104), [128, 8, 8])
        for tti, (t0, n) in enumerate(TT):
            ps, psd = k.psum()
            for kc in range(8):
                k.mm(ps[0:4, 0:n], Wgt[:, kc, 0:4], xn[:, kc, t0:t0 + n], kc == 0, kc == 7, R=(Wgtd, xn_d[tti]), W=psd, inc=(kc == 7))
            k.op(ACT, lambda: A.activation(out=GI[:, t0:t0 + n], in_=ps[0:4, 0:n], func=AF.Identity, bias=gb[:, j:j + 1]),
                 R=psd + [const_dep], W=(GI_d,))
            ps, psd = k.psum()
            for kc in range(8):
                k.mm(ps[0:4, 0:n], Wgt[:, kc, 4:8], xn[:, kc, t0:t0 + n], kc == 0, kc == 7, R=(Wgtd, xn_d[tti]), W=psd, inc=(kc == 7))
            k.op(ACT, lambda: A.activation(out=GL[:, t0:t0 + n], in_=ps[0:4, 0:n], func=AF.Exp, scale=-1.0, bias=gb[:, 4 + j:5 + j]),
                 R=psd + [const_dep], W=(GL_d,))
        k.op(ACT, lambda: A.activation(out=GL, in_=GL, func=AF.Ln, bias=1.0), W=(GL_d,))
        k.op(DVE, lambda: V.tensor_tensor_scan(out=GB, data0=RM, data1=GL, initial=0.0, op0=ALU.mult, op1=ALU.add),
             R=(RM_d, GL_d), W=(GB_d,))
        k.op(DVE, lambda: V.tensor_tensor(out=GI, in0=GI, in1=GB, op=ALU.add), R=(GB_d,), W=(GI_d,))
        GIp = GI[:, 0:NPR].rearrange("p (c t) -> p c t", t=128)
        GIs = GI[:, NPR:NT].rearrange("p (c t) -> p c t", t=4)
        GBp = GB[:, 0:NPR].rearrange("p (c t) -> p c t", t=128)
        GBs = GB[:, NPR:NT].rearrange("p (c t) -> p c t", t=4)
        k.op(DVE, lambda: V.reduce_max(out=GM[:, 0:16], in_=GIp, axis=AX.X), R=(GI_d,), W=(sml_d,))
        k.op(DVE, lambda: V.reduce_max(out=GM[:, 16:32], in_=GIs, axis=AX.X), R=(GI_d,), W=(sml_d,))
        k.op(DVE, lambda: V.tensor_scalar_mul(out=BL[:, 0:16], in0=GBp[:, :, 127], scalar1=-1.0), R=(GB_d,), W=(sml_d,))
        k.op(DVE, lambda: V.tensor_scalar_mul(out=BL[:, 16:32], in0=GBs[:, :, 3], scalar1=-1.0), R=(GB_d,), W=(sml_d,))
        k.op(DVE, lambda: V.memset(MM[:, 0:1], 0.0), W=(sml_d,))
        k.op(DVE, lambda: V.tensor_tensor_scan(out=MM[:, 1:17], data0=GM[:, 0:16], data1=BL[:, 0:16], initial=0.0,
                                               op0=ALU.max, op1=ALU.add), W=(sml_d,))
        k.op(DVE, lambda: V.tensor_tensor(out=CC[:, 0:16], in0=MM[:, 0:16], in1=GM[:, 0:16], op=ALU.max), W=(sml_d,))
        k.op(DVE, lambda: V.tensor_tensor(out=CC[:, 16:32], in0=MS, in1=GM[:, 16:32], op=ALU.max), W=(sml_d,))
        k.op(DVE, lambda: V.tensor_tensor(out=MN, in0=CC[:, 16:32], in1=BL[:, 16:32], op=ALU.add), W=(sml_d,))
        k.op(DVE, lambda: V.tensor_tensor(out=DEC[:, 0:16], in0=MM[:, 0:16], in1=CC[:, 0:16], op=ALU.subtract), W=(sml_d,))
        k.op(DVE, lambda: V.tensor_tensor(out=DEC[:, 16:32], in0=MS, in1=CC[:, 16:32], op=ALU.subtract), W=(sml_d,))
        k.op(ACT, lambda: A.activation(out=DEC, in_=DEC, func=AF.Exp), W=(sml_d,))
        k.dma(SP, mp_d[j].rearrange("(h o) -> h o", o=1), MM[:, 16:17], R=(sml_d,))
        k.dma(SP, ms_d[j].rearrange("b h -> h b"), MN, R=(sml_d,), allow_slow_non_contiguous=True)
        for (Xp, Xs, Xd) in ((GIp, GIs, GI_d), (GBp, GBs, GB_d)):
            k.op(DVE, lambda: V.tensor_tensor(out=Xp, in0=Xp, in1=CC[:, 0:16].unsqueeze(2).to_broadcast([4, 16, 128]), op=ALU.subtract),
                 R=(sml_d,), W=(Xd,))
            k.op(DVE, lambda: V.tensor_tensor(out=Xs, in0=Xs, in1=CC[:, 16:32].unsqueeze(2).to_broadcast([4, 16, 4]), op=ALU.subtract),
                 R=(sml_d,), W=(Xd,))
        k.op(ACT, lambda: A.activation(out=GI, in_=GI, func=AF.Exp), W=(GI_d,))
        k.op(ACT, lambda: A.activation(out=GB, in_=GB, func=AF.Exp), W=(GB_d,))
        pT, pTd = k.psum()
        for tt in range(17):
            n = 128 if tt < 16 else 64
            k.tr(pT[0:n, tt * 8:tt * 8 + 4], GI[:, tt * 128:tt * 128 + n], ident[0:4, 0:4], R=(GI_d, cst_dep), W=pTd, inc=False)
            k.tr(pT[0:n, tt * 8 + 4:tt * 8 + 8], GB[:, tt * 128:tt * 128 + n], ident[0:4, 0:4], R=(GB_d, cst_dep), W=pTd, inc=(tt == 16))
        k.op(DVE, lambda: V.tensor_copy(out=WT[:, 0:16, :], in_=pT[:, 0:128].rearrange("p (t c) -> p t c", c=8)), R=pTd, W=(WT_d,))
        k.op(DVE, lambda: V.tensor_copy(out=WT[0:64, 16, :], in_=pT[0:64, 128:136]), R=pTd, W=(WT_d,))
        k.op(DVE, lambda: V.tensor_tensor(out=DDt[:].rearrange("p (g h) -> p g h", h=4), in0=DEC.unsqueeze(2).to_broadcast([4, 32, 4]),
                                          in1=ident[0:4, 0:4].unsqueeze(1).to_broadcast([4, 32, 4]), op=ALU.mult),
             R=(sml_d, cst_dep), W=(sml_d,))
        pD, pDd = k.psum()
        k.mm(pD[:, 0:128], ones4[:], DDt[:], True, True, R=(sml_d, const_dep), W=pDd, inc=True)
        k.op(DVE, lambda: V.tensor_copy(out=DECB[:], in_=pD[:, 0:128]), R=pDd, W=(DECB_d,))
        o_ = 0
        qT = arena[:, o_:o_ + 2 * NT].rearrange("p (c t) -> p c t", c=2); o_ += 2 * NT
        kT = arena[:, o_:o_ + 2 * NT].rearrange("p (c t) -> p c t", c=2); o_ += 2 * NT
        soT = arena[:, o_:o_ + 2 * NT].rearrange("p (c t) -> p c t", c=2); o_ += 2 * NT
        VX = arena[:, o_:o_ + 17 * 257].rearrange("p (t e) -> p t e", t=17); o_ += 17 * 257 + 1
        KW = arena[:, o_:o_ + 17 * 256].rearrange("p (t e) -> p t e", t=17); o_ += 17 * 256
        HN = arena[:, o_:o_ + 17 * 256].rearrange("p (t e) -> p t e", t=17); o_ += 17 * 256
        assert o_ <= ARENA_EL
        dl = arena_deps([6, 17])
        qT_d, kT_d, so_d = dl[0][0:5], dl[1][0:5], dl[2][0:5]
        VX_d, KW_d, HN_d = dl[3], dl[4], dl[5]
        for tt in range(17):
            k.op(DVE, lambda: V.memset(VX[:, tt, 256:257], 1.0), W=(VX_d[tt],))
        tile_of = lambda tt: (tt * 128, 128 if tt < 16 else 64, min(tt // 4, 4))
        for h in range(4):
            Wq, Wqd = k.wload(rows_kc(win, h * 256, (h + 1) * 256), [128, 8, 256])
            for tti, (t0, n) in enumerate(TT):
                for dc in range(2):
                    ps, psd = k.psum()
                    for kc in range(8):
                        k.mm(ps[:, 0:n], Wq[:, kc, dc * 128:(dc + 1) * 128], xn[:, kc, t0:t0 + n], kc == 0, kc == 7,
                             R=(Wqd, xn_d[tti]), W=psd, inc=(kc == 7))
                    k.op(ACT, lambda: A.copy(out=qT[:, dc, t0:t0 + n], in_=ps[:, 0:n]), R=psd, W=(qT_d[tti],))
            Wk, Wkd = k.wload(rows_kc(win, 1024 + h * 256, 1024 + (h + 1) * 256), [128, 8, 256])
            for tti, (t0, n) in enumerate(TT):
                for dc in range(2):
                    ps, psd = k.psum()
                    for kc in range(8):
                        k.mm(ps[:, 0:n], Wk[:, kc, dc * 128:(dc + 1) * 128], xn[:, kc, t0:t0 + n], kc == 0, kc == 7,
                             R=(Wkd, xn_d[tti]), W=psd, inc=(kc == 7))
                    k.op(ACT, lambda: A.mul(out=kT[:, dc, t0:t0 + n], in_=ps[:, 0:n], mul=1.0 / 16.0), R=psd, W=(kT_d[tti],))
            for tt in range(17):
                c0, n, tti = tile_of(tt)
                ps, psd = k.psum()
                for kc in range(8):
                    k.mm(ps[0:n, 0:256], xn[:, kc, c0:c0 + n], Wk[:, kc, :], kc == 0, kc == 7, R=(Wkd, xn_d[tti]), W=psd, inc=(kc == 7))
                k.op(DVE, lambda: V.tensor_scalar(out=KW[0:n, tt, :], in0=ps[0:n, 0:256], scalar1=WT[0:n, tt, h:h + 1], scalar2=1.0 / 16.0,
                                                  op0=ALU.mult, op1=ALU.mult), R=psd + [WT_d], W=(KW_d[tt],))
            Wv, Wvd = k.wload(rows_kc(win, 2048 + h * 256, 2048 + (h + 1) * 256), [128, 8, 256])
            for tt in range(17):
                c0, n, tti = tile_of(tt)
                ps, psd = k.psum()
                for kc in range(8):
                    k.mm(ps[0:n, 0:256], xn[:, kc, c0:c0 + n], Wv[:, kc, :], kc == 0, kc == 7, R=(Wvd, xn_d[tti]), W=psd, inc=(kc == 7))
                k.op(ACT, lambda: A.copy(out=VX[0:n, tt, 0:256], in_=ps[0:n, 0:256]), R=psd, W=(VX_d[tt],))
            Wo_, Wod_ = k.wload(rows_kc(win, 3072 + h * 256, 3072 + (h + 1) * 256), [128, 8, 256])
            for tti, (t0, n) in enumerate(TT):
                for dc in range(2):
                    ps, psd = k.psum()
                    for kc in range(8):
                        k.mm(ps[:, 0:n], Wo_[:, kc, dc * 128:(dc + 1) * 128], xn[:, kc, t0:t0 + n], kc == 0, kc == 7,
                             R=(Wod_, xn_d[tti]), W=psd, inc=(kc == 7))
                    k.op(ACT, lambda: A.activation(out=soT[:, dc, t0:t0 + n], in_=ps[:, 0:n], func=AF.Sigmoid), R=psd, W=(so_d[tti],))
            def stage_a(j2):
                c0 = j2 * 128
                tti = j2 // 4
                pS, pSd = k.psum()
                for dc in range(2):
                    k.mm(pS[:, 0:128], kT[:, dc, c0:c0 + 128], qT[:, dc, c0:c0 + 128], dc == 0, dc == 1,
                         R=(kT_d[tti], qT_d[tti]), W=pSd, inc=(dc == 1))
                SpT, sptd = sptb[:, j2 % 2, :], sptb_d[j2 % 2]
                k.op(DVE, lambda: V.scalar_tensor_tensor(out=SpT, in0=pS[:, 0:128], scalar=WT[:, j2, h:h + 1], in1=Ub[:],
                                                         op0=ALU.mult, op1=ALU.mult), R=pSd + [WT_d, const_dep], W=(sptd,))
                pC, pCd = k.psum(2)
                for dc in range(2):
                    k.mm(pC[:, dc, 0:257], KW[:, j2, dc * 128:(dc + 1) * 128], VX[:, j2, :], True, True,
                         R=(KW_d[j2], VX_d[j2]), W=pCd, inc=(dc == 1))
                return SpT, sptd, pC, pCd

            cb_next = {}

            def stage_b(j2, st_a):
                SpT, sptd, pC, pCd = st_a
                c0 = j2 * 128
                tti = j2 // 4
                dcol = DECB[:, j2 * 4 + h:j2 * 4 + h + 1]
                if j2 == 0:
                    k.op(DVE, lambda: V.tensor_copy(out=Cst[:], in_=pC[:, :, 0:257]), R=pCd, W=(Cst_d,))
                else:
                    k.op(DVE, lambda: V.scalar_tensor_tensor(out=Cst[:], in0=Cst[:], scalar=dcol, in1=pC[:, :, 0:257],
                                                             op0=ALU.mult, op1=ALU.add), R=pCd + [DECB_d], W=(Cst_d,))
                if j2 > 0:
                    CB, cbd = cb_next[j2]
                if j2 + 1 < 16:
                    dnx = DECB[:, (j2 + 1) * 4 + h:(j2 + 1) * 4 + h + 1]
                    cbt, cbd_n = k.slot()
                    CBn = cbt[:, 0:514].rearrange("p (c e) -> p c e", c=2)
                    k.op(ACT, lambda: A.mul(out=CBn, in_=Cst[:], mul=dnx), R=(Cst_d, DECB_d), W=(cbd_n,))
                    cb_next[j2 + 1] = (CBn, cbd_n)
                pN, pNd = k.psum()
                k.mm(pN[:, 0:257], SpT, VX[:, j2, :], True, j2 == 0, R=(sptd, VX_d[j2]), W=pNd, inc=(j2 == 0))
                if j2 > 0:
                    for dc in range(2):
                        k.mm(pN[:, 0:257], qT[:, dc, c0:c0 + 128], CB[:, dc, :], False, dc == 1, R=(qT_d[tti], cbd), W=pNd, inc=(dc == 1))
                rr_ = RR[:, j2:j2 + 1]
                k.op(DVE, lambda: V.tensor_scalar_mul(out=rr_, in0=pN[:, 256:257], scalar1=-1.0), R=pNd, W=(RR_d,))
                k.op(DVE, lambda: V.tensor_tensor(out=rr_, in0=pN[:, 256:257], in1=rr_, op=ALU.max), R=pNd, W=(RR_d,))
                k.op(DVE, lambda: V.tensor_tensor(out=rr_, in0=rr_, in1=WT[:, j2, 4 + h:5 + h], op=ALU.max), R=(WT_d,), W=(RR_d,))
                k.op(DVE, lambda: V.reciprocal(out=rr_, in_=rr_), W=(RR_d,))
                k.op(ACT, lambda: A.mul(out=HN[:, j2, :], in_=pN[:, 0:256], mul=rr_), R=pNd + [RR_d], W=(HN_d[j2],))

            sc0 = NPR
            pS, pSd = k.psum()
            for dc in range(2):
                k.mm(pS[0:64, 0:64], kT[:, dc, sc0:NT], qT[:, dc, sc0:NT], dc == 0, dc == 1, R=(kT_d[4], qT_d[4]), W=pSd, inc=(dc == 1))
            spt, sptd = k.slot()
            SpTs = spt[0:64, 0:64]
            k.op(DVE, lambda: V.scalar_tensor_tensor(out=SpTs, in0=pS[0:64, 0:64], scalar=WT[0:64, 16, h:h + 1], in1=maskbd[:],
                                                     op0=ALU.mult, op1=ALU.mult), R=pSd + [WT_d, const_dep], W=(sptd,))
            pN, pNd = k.psum()
            k.mm(pN[0:64, 0:257], SpTs, VX[0:64, 16, :], True, True, R=(sptd, VX_d[16]), W=pNd, inc=True)
            k.op(DVE, lambda: V.tensor_copy(out=numS[:], in_=pN[0:64, 0:257]), R=pNd, W=(numS_d,))
            st_next = stage_a(0)

            pDN, pDNd = k.psum()
            for dc in range(2):
                k.mm(pDN[:, dc * 16:(dc + 1) * 16], KW[0:64, 16, dc * 128:(dc + 1) * 128], rmb[:], True, True,
                     R=(KW_d[16], const_dep), W=pDNd, inc=(dc == 1))
            for dc in range(2):
                nv = NSTn[:, dc, :].rearrange("p (b h) -> p b h", h=4)[:, :, h]
                k.op(DVE, lambda: V.tensor_tensor(out=nv, in0=NST[:, dc, :].rearrange("p (b h) -> p b h", h=4)[:, :, h],
                                                  in1=DECB[:, 64:128].rearrange("p (b h) -> p b h", h=4)[:, :, h], op=ALU.mult),
                     R=(NST_d, DECB_d), W=(NSTn_d,))
                k.op(DVE, lambda: V.tensor_tensor(out=nv, in0=pDN[:, dc * 16:(dc + 1) * 16], in1=nv, op=ALU.add), R=pDNd, W=(NSTn_d,))

            def sample_front(b):
                dcol = DECB[:, (16 + b) * 4 + h:(16 + b) * 4 + h + 1]
                rmk = cst[0:64, 320 + b:321 + b]
                cbt_, cbd_ = k.slot()
                Cb = cbt_.bitcast(F32)[:, 0:514].rearrange("p (c e) -> p c e", c=2)
                k.dma(SP, Cb[:, :, 0:256], sC_d[j, b, h].rearrange("(dc p) e -> p dc e", p=128), W=(cbd_,))
                k.op(ACT, lambda: A.copy(out=Cb[:, :, 256:257], in_=NST[:, :, b * 4 + h:b * 4 + h + 1]), R=(NST_d,), W=(cbd_,))
                cbb, cbbd = k.slot()
                CBb = cbb[:, 0:514].rearrange("p (c e) -> p c e", c=2)
                k.op(ACT, lambda: A.mul(out=CBb, in_=Cb, mul=dcol), R=(cbd_, DECB_d), W=(cbbd,))
                vxt, vxd = k.slot()
                VXb = vxt[0:64, 0:256]
                k.op(ACT, lambda: A.mul(out=VXb, in_=VX[0:64, 16, 0:256], mul=rmk), R=(VX_d[16], cst_dep), W=(vxd,))
                return (Cb, cbd_, CBb, cbbd, VXb, vxd, dcol, rmk)

            def sample_back(b, fr):
                Cb, cbd_, CBb, cbbd, VXb, vxd, dcol, rmk = fr
                pb, pbd = k.psum_pin(0)
                for dc in range(2):
                    k.mm(pb[0:64, 0:257], qT[:, dc, sc0:NT], CBb[:, dc, :], dc == 0, dc == 1, R=(qT_d[4], cbbd), W=pbd, inc=(dc == 1))
                k.op(DVE, lambda: V.scalar_tensor_tensor(out=numS[:], in0=pb[0:64, 0:257], scalar=rmk, in1=numS[:],
                                                         op0=ALU.mult, op1=ALU.add), R=pbd + [cst_dep], W=(numS_d,))
                pC1, pC1d = k.psum_pin(1)
                for dc in range(2):
                    k.mm(pC1[:, dc * 256:(dc + 1) * 256], KW[0:64, 16, dc * 128:(dc + 1) * 128], VXb, True, True,
                         R=(KW_d[16], vxd), W=pC1d, inc=(dc == 1))
                cnt_, cnd_ = k.slot()
                Cn = cnt_.bitcast(F32)[:, 0:512].rearrange("p (c e) -> p c e", c=2)
                k.op(DVE, lambda: V.scalar_tensor_tensor(out=Cn, in0=Cb[:, :, 0:256], scalar=dcol, in1=pC1.rearrange("p (c e) -> p c e", c=2),
                                                         op0=ALU.mult, op1=ALU.add), R=pC1d + [cbd_, DECB_d], W=(cnd_,))
                k.dma(POOL, Cs_d[j, b, h].rearrange("(dc p) e -> p dc e", p=128), Cn, R=(cnd_,))

            fr_prev = None
            for j2 in range(16):
                st_cur = st_next
                if j2 + 1 < 16:
                    st_next = stage_a(j2 + 1)
                if fr_prev is not None:
                    sample_back(j2 - 1, fr_prev)
                fr_prev = sample_front(j2)
                stage_b(j2, st_cur)
            sample_back(15, fr_prev)
            k.dma(POOL, Cp_d[j, h].rearrange("(dc p) e -> p dc e", p=128), Cst[:, :, 0:256], R=(Cst_d,))
            k.dma(POOL, np_d[j, h].rearrange("(dc p o) -> p dc o", p=128, o=1), Cst[:, :, 256:257], R=(Cst_d,), allow_slow_non_contiguous=True)
            rs_ = RR[0:64, 16:17]
            k.op(DVE, lambda: V.tensor_scalar_mul(out=rs_, in0=numS[:, 256:257], scalar1=-1.0), R=(numS_d,), W=(RR_d,))
            k.op(DVE, lambda: V.tensor_tensor(out=rs_, in0=numS[:, 256:257], in1=rs_, op=ALU.max), R=(numS_d,), W=(RR_d,))
            k.op(DVE, lambda: V.tensor_tensor(out=rs_, in0=rs_, in1=WT[0:64, 16, 4 + h:5 + h], op=ALU.max), R=(WT_d,), W=(RR_d,))
            k.op(DVE, lambda: V.reciprocal(out=RR[0:64, 16:17], in_=RR[0:64, 16:17]), W=(RR_d,))
            k.op(ACT, lambda: A.mul(out=HN[0:64, 16, :], in_=numS[:, 0:256], mul=RR[0:64, 16:17]), R=(numS_d, RR_d), W=(HN_d[16],))
            k.op(DVE, lambda: V.memset(SQ[:], 0.0), W=(SQ_d,))
            for tt in range(17):
                n = 128 if tt < 16 else 64
                jt, jtd = k.slot()
                jb = jt[0:n, 0:256]
                k.op(DVE, lambda: V.scalar_tensor_tensor(out=jb, in0=HN[0:n, tt, :], scalar=1.0, in1=HN[0:n, tt, :], op0=ALU.mult, op1=ALU.mult,
                                                         accum_out=SQ[0:n, tt:tt + 1]), R=(HN_d[tt],), W=(jtd, SQ_d))
            for (r0, r1, c0_, c1_) in ((0, 128, 0, 16), (0, 64, 16, 17)):
                sq = SQ[r0:r1, c0_:c1_]
                k.op(DVE, lambda: V.tensor_scalar(out=sq, in0=sq, scalar1=1.0 / 256.0, scalar2=EPS, op0=ALU.mult, op1=ALU.add), W=(SQ_d,))
                k.op(ACT, lambda: A.sqrt(out=sq, in_=sq), W=(SQ_d,))
                k.op(DVE, lambda: V.reciprocal(out=sq, in_=sq), W=(SQ_d,))
            for ec in range(2):
                gh = VEC[:, 152 + j * 8 + h * 2 + ec:153 + j * 8 + h * 2 + ec]
                for grp in range(5):
                    tts = list(range(grp * 4, grp * 4 + 4)) if grp < 4 else [16]
                    pt, ptd = k.psum()
                    ptb = pt.bitcast(BF16)
                    hs = []
                    for idx, tt in enumerate(tts):
                        n = 128 if tt < 16 else 64
                        hsl, hsd = k.slot()
                        hv = hsl[0:n, 0:128]
                        k.op(ACT, lambda: A.mul(out=hv, in_=HN[0:n, tt, ec * 128:(ec + 1) * 128], mul=SQ[0:n, tt:tt + 1]),
                             R=(HN_d[tt], SQ_d), W=(hsd,))
                        k.tr(ptb[:, idx * 128:idx * 128 + n], hv, identb[0:n, 0:n], R=(hsd, const_dep), W=ptd, inc=(idx == len(tts) - 1))
                    t0, n = TT[grp]
                    k.op(DVE, lambda: V.scalar_tensor_tensor(out=soT[:, ec, t0:t0 + n], in0=ptb[:, 0:n], scalar=gh, in1=soT[:, ec, t0:t0 + n],
                                                             op0=ALU.mult, op1=ALU.mult), R=ptd + [const_dep], W=(so_d[grp],))
            Wout, Woutd = k.wload(mw_out_d[j][h * 256:(h + 1) * 256, :].rearrange("(a p) n -> p a n", p=128), [128, 2, 1024])
            for tti, (t0, n) in enumerate(TT):
                for m in range(8):
                    ps, psd = k.psum()
                    for dc in range(2):
                        k.mm(ps[:, 0:n], Wout[:, dc, m * 128:(m + 1) * 128], soT[:, dc, t0:t0 + n], dc == 0, dc == 1,
                             R=(Woutd, so_d[tti]), W=psd, inc=(dc == 1))
                    k.op(DVE, lambda: V.tensor_tensor(out=xres[:, m, t0:t0 + n], in0=ps[:, 0:n], in1=xres[:, m, t0:t0 + n], op=ALU.add),
                         R=psd, W=(xres_d[m][tti],))
                if h == 3:
                    early_norm(tti)
        pt, ptd = k.psum()
        for dc in range(2):
            k.tr(pt[0:64, dc * 128:(dc + 1) * 128], NSTn[:, dc, :], ident, R=(NSTn_d, cst_dep), W=ptd, inc=(dc == 1))
        st, sd = k.slot()
        s32 = st.bitcast(F32)
        k.op(DVE, lambda: V.tensor_copy(out=s32[0:64, 0:256], in_=pt[0:64, 0:256]), R=ptd, W=(sd,))
        k.dma(SP, ns_d[j], s32[0:64, 0:256], R=(sd,))

    subs = []
    for l in range(nd):
        if FLAGS["ffn"]:
            subs.append((lambda l=l: ffn(l, 0), (l, 0)))
        if FLAGS["mix"] and l % 2 == 0 and FLAGS.get("mlstm", True):
            subs.append((lambda l=l: mlstm_mixer(l), (l, 1)))
        if FLAGS["mix"] and l % 2 == 1 and FLAGS.get("pool", True):
            subs.append((lambda l=l: pool_mixer(l), (l, 1)))
        if FLAGS["xattn"]:
            subs.append((lambda l=l: xattn(l), (l, 2)))
        if FLAGS["ffn"]:
            subs.append((lambda l=l: ffn(l, 1), (l, 3)))
    for si, (fn, nid) in enumerate(subs):
        norm_state["next"] = subs[si + 1][1] if si + 1 < len(subs) else None
        fn()

    fin = arena[:, 0:2 * 8 * 512].bitcast(F32).rearrange("p (c t) -> p c t", c=8)
    flush_pending()
    fin_d = arena_deps([1])[0]
    for tti, (t0, n) in enumerate(TT):
        rmsnorm_tile(tti, lambda c: VEC[:, 128 + c:129 + c], lambda c: (fin[:, c, 0:n], (fin_d,)), None)
        for s in range((n + 127) // 128):
            nr = min(128, n - s * 128)
            ost, osd = k.slot()
            ost2, osd2 = k.slot()
            for hlf, (o_, od_) in enumerate(((ost, osd), (ost2, osd2))):
                o32 = o_.bitcast(F32)
                pt, ptd = k.psum()
                for c4 in range(4):
                    k.tr(pt[0:nr, c4 * 128:(c4 + 1) * 128], fin[:, hlf * 4 + c4, s * 128:s * 128 + nr], ident,
                         R=(fin_d, cst_dep), W=ptd, inc=(c4 == 3))
                if hlf == 0:
                    k.op(ACT, lambda: A.copy(out=o32[0:nr, 0:512], in_=pt[0:nr, :]), R=ptd, W=(od_,))
                else:
                    k.op(DVE, lambda: V.tensor_copy(out=o32[0:nr, 0:512], in_=pt[0:nr, :]), R=ptd, W=(od_,))
                r0 = t0 + s * 128
                dst = yp_d[r0:r0 + nr, hlf * 512:(hlf + 1) * 512] if r0 < NPR else ys_d[0:nr, hlf * 512:(hlf + 1) * 512]
                k.dma(SP, dst, o32[0:nr, 0:512], R=(od_,))
    k.finish()
    return nc


_OUT_NAMES = ["yp", "ys", "mk", "mv", "Cp", "np_", "mp", "Cs", "ns", "ms", "pbp", "pbs"]


def _consts():
    cst = np.zeros((128, 512), np.float32)
    cst[:, 0:128] = np.eye(128, dtype=np.float32)
    s = np.arange(128)
    cst[:, 128:256] = (s[:, None] <= s[None, :]).astype(np.float32)
    s64 = np.arange(64)
    cst[0:64, 256:320] = ((s64[:, None] // 4 == s64[None, :] // 4) & (s64[:, None] <= s64[None, :])).astype(np.float32)
    cst[0:64, 320:336] = (s64[:, None] // 4 == np.arange(16)[None, :]).astype(np.float32)
    for g, w in enumerate((2, 4, 8, 16)):
        cst[:, 384 + g * 16:384 + (g + 1) * 16] = 1.0 / np.minimum(np.arange(16) + 1, w)
    cst[:, 448] = EPS
    rm = np.ones((4, NT), np.float32)
    rm[:, 0:NPR:128] = 0.0
    rm[:, NPR::4] = 0.0
    return cst, rm


def kernel(**inp):
    f = lambda a: np.ascontiguousarray(np.asarray(a, dtype=np.float32))
    cst, rm = _consts()
    vecs = np.zeros((256, 128), np.float32)
    vecs[0:128] = f(inp["norm_g"]).reshape(128, 128)
    vecs[128:136] = f(inp["final_g"]).reshape(8, 128)
    vecs[136:152] = f(inp["pool_scale"]).reshape(16, 128)
    vecs[152:168] = f(inp["mlstm_g_head"]).reshape(16, 128)
    gb = np.ascontiguousarray(np.concatenate([f(inp["mlstm_b_i"]).T, f(inp["mlstm_b_f"]).T], axis=1))
    shared = {n: f(inp[n]) for n in ("ffn_w_up", "ffn_w_down", "mlstm_w_in", "mlstm_w_out", "pool_w_in", "pool_w_grp",
                                      "pool_w_out", "xattn_w_q", "xattn_w_kv", "xattn_w_o")}
    shared.update(vecs=vecs, gb=gb, cst=cst, rm=rm)
    in_maps = []
    for c in range(8):
        b0 = 16 * c
        m = dict(shared)
        m["xp"] = f(inp["x_prompt"][c])
        m["xs"] = f(inp["x_sample"][b0:b0 + 16]).reshape(NSM, D)
        m["mem"] = f(inp["mem_prompt"][c])
        m["ck"] = f(inp["cache_mem_k"][:, b0:b0 + 16]).reshape(DEPTH, 16, NMEM, D)
        m["cv"] = f(inp["cache_mem_v"][:, b0:b0 + 16]).reshape(DEPTH, 16, NMEM, D)
        m["sC"] = f(inp["state_mlstm_C"][:, b0:b0 + 16])
        m["sn"] = f(inp["state_mlstm_n"][:, b0:b0 + 16]).reshape(2, 64, 256)
        m["sm"] = f(inp["state_mlstm_m"][:, b0:b0 + 16])
        m["spb"] = f(inp["state_pool_buf"][:, b0:b0 + 16]).reshape(2, 240, D)
        in_maps.append(m)
    nc = build()
    res = run_bass_kernel_spmd(nc, in_maps, core_ids=list(range(8)))
    R = res.results
    g = lambda n: [np.asarray(R[c][n]) for c in range(8)]
    y_prompt = np.stack(g("yp"), 0)
    y_sample = np.concatenate([a.reshape(16, 4, D) for a in g("ys")], 0)
    mk = np.stack(g("mk"), 1).reshape(DEPTH, 8, NMEM, 4, 256)
    mv = np.stack(g("mv"), 1).reshape(DEPTH, 8, NMEM, 4, 256)
    Cp = np.stack(g("Cp"), 1)
    np_ = np.stack(g("np_"), 1)
    mp = np.stack(g("mp"), 1)
    Cs = np.concatenate(g("Cs"), 1)
    ns = np.concatenate([a.reshape(2, 16, 4, 256) for a in g("ns")], 1)
    ms = np.concatenate(g("ms"), 1)
    pbp = np.stack(g("pbp"), 1)
    pbs = np.concatenate(g("pbs"), 1)
    return (y_prompt, y_sample, mk, mv, Cp, np_, mp, Cs, ns, ms, pbp, pbs)
```
